# Optimizing a Trainium2 kernel written in Bass

```python
import math
import jax, jax.numpy as jnp
from jax import lax
import numpy as np

D_MODEL = 1024
BATCH = 16
SEQ = 4096
DEPTH = 1

ATTN_WIDTH = D_MODEL // 2
LRU_WIDTH = D_MODEL - ATTN_WIDTH
ATTN_DK = 64
ATTN_DV = 2 * ATTN_DK
ATTN_HEADS = ATTN_WIDTH // ATTN_DV
QK_WIDTH = ATTN_HEADS * 2 * ATTN_DK
LRU_BLOCKS = 8
LRU_BW = LRU_WIDTH // LRU_BLOCKS
CONV_W = 4
LRU_C = 8.0
D_IN_PROJ = 2 * QK_WIDTH + ATTN_WIDTH + 2 * LRU_WIDTH
Q_BLOCK = 128
N_EXPERTS = 32
TOP_K = 4
D_FF = D_MODEL
SWIGLU_LIMIT = 7.0
SWIGLU_ALPHA = 1.702
MOE_BLOCK = 256
EPS = 1e-6

kernel_name = "hymba_diffattn_rglru_moe_adaln"


def rms_norm(x, g):
    xf = x.astype(jnp.float32)
    y = xf * lax.rsqrt(jnp.mean(xf * xf, axis=-1, keepdims=True) + EPS)
    return (y * g.astype(jnp.float32)).astype(x.dtype)


def modulate(h, shift, scale):
    return h * (1.0 + scale[:, None, :]) + shift[:, None, :]


def causal_diff_attention(q, k, v, lam):
    B, S, H, _, dk = q.shape
    n_blocks = S // Q_BLOCK
    scale = dk ** -0.5
    kpos = jnp.arange(S)

    def block(i):
        start = i * Q_BLOCK
        qb = lax.dynamic_slice_in_dim(q, start, Q_BLOCK, axis=1)
        s = jnp.einsum('bqhcd,bkhcd->bhcqk', qb, k).astype(jnp.float32) * scale
        qpos = start + jnp.arange(Q_BLOCK)
        mask = kpos[None, :] <= qpos[:, None]
        s = jnp.where(mask, s, -jnp.inf)
        p = jax.nn.softmax(s, axis=-1)
        a = p[:, :, 0] - lam * p[:, :, 1]
        return jnp.einsum('bhqk,bkhd->bqhd', a.astype(v.dtype), v)

    o = lax.map(block, jnp.arange(n_blocks))
    return o.transpose(1, 0, 2, 3, 4).reshape(B, S, H, v.shape[-1])


def _linear_scan_combine(left, right):
    a1, b1 = left
    a2, b2 = right
    return a1 * a2, a2 * b1 + b2


def rg_lru_branch(xr, gr, conv_w, conv_b, wa, ba, wx, bx, lru_lambda):
    B, S, C = xr.shape
    xc = lax.conv_general_dilated(
        xr, conv_w[:, None, :].astype(xr.dtype), window_strides=(1,),
        padding=[(CONV_W - 1, 0)], dimension_numbers=('NWC', 'WIO', 'NWC'),
        feature_group_count=C) + conv_b
    xb = xc.reshape(B, S, LRU_BLOCKS, LRU_BW)
    r = jax.nn.sigmoid(jnp.einsum('bsni,nij->bsnj', xb, wa).reshape(B, S, C) + ba)
    i = jax.nn.sigmoid(jnp.einsum('bsni,nij->bsnj', xb, wx).reshape(B, S, C) + bx)
    log_a = -LRU_C * r.astype(jnp.float32) * jax.nn.softplus(-lru_lambda.astype(jnp.float32))
    a = jnp.exp(log_a)
    b = jnp.sqrt(-jnp.expm1(2.0 * log_a)) * (i * xc).astype(jnp.float32)
    _, h = lax.associative_scan(_linear_scan_combine, (a, b), axis=1)
    return h.astype(xr.dtype) * jax.nn.gelu(gr, approximate=True)


def moe_ffn(h, router_w, router_b, w_gu, b_gu, w_dn, b_dn):
    B, S, D = h.shape
    T = B * S
    xt = h.reshape(T, D)
    logits = (xt @ router_w + router_b).astype(jnp.float32)
    top_v, top_i = lax.top_k(logits, TOP_K)
    gates = jax.nn.softmax(top_v, axis=-1)
    N = T * TOP_K
    e_flat = top_i.reshape(N)
    tok_flat = jnp.arange(N, dtype=jnp.int32) // TOP_K
    g_flat = gates.reshape(N)
    order = jnp.argsort(e_flat)
    e_s, tok_s, g_s = e_flat[order], tok_flat[order], g_flat[order]
    counts = jnp.bincount(e_flat, length=N_EXPERTS)
    padded = ((counts + MOE_BLOCK - 1) // MOE_BLOCK) * MOE_BLOCK
    starts = jnp.cumsum(counts) - counts
    pad_ends = jnp.cumsum(padded)
    pad_starts = pad_ends - padded
    dest = pad_starts[e_s] + (jnp.arange(N) - starts[e_s])
    n_blocks = -(-N // MOE_BLOCK) + N_EXPERTS
    P = n_blocks * MOE_BLOCK
    slot_tok = jnp.full((P,), T, dtype=jnp.int32).at[dest].set(tok_s)
    slot_gate = jnp.zeros((P,), jnp.float32).at[dest].set(g_s)
    block_expert = jnp.minimum(
        jnp.searchsorted(pad_ends, jnp.arange(n_blocks) * MOE_BLOCK, side='right'),
        N_EXPERTS - 1).astype(jnp.int32)
    x_pad = jnp.concatenate([xt, jnp.zeros((1, D), xt.dtype)], axis=0)

    def run_block(args):
        tok_b, e = args
        xb = x_pad[tok_b]
        gu = xb @ w_gu[e] + b_gu[e]
        gate, up = jnp.split(gu, 2, axis=-1)
        gate = jnp.minimum(gate, SWIGLU_LIMIT)
        up = jnp.clip(up, -SWIGLU_LIMIT, SWIGLU_LIMIT)
        act = (up + 1.0) * (gate * jax.nn.sigmoid(SWIGLU_ALPHA * gate))
        return act @ w_dn[e] + b_dn[e]

    y_slots = lax.map(run_block, (slot_tok.reshape(n_blocks, MOE_BLOCK), block_expert))
    y_slots = y_slots.reshape(P, D) * slot_gate[:, None].astype(xt.dtype)
    y = jnp.zeros((T + 1, D), xt.dtype).at[slot_tok].add(y_slots)
    return y[:T].reshape(B, S, D)


def setup_inputs(seed: int = 0) -> dict:
    key = jax.random.key(seed)
    ks = jax.random.split(key, 32)

    def nrm(k, shape, scale):
        return scale * jax.random.normal(k, shape, jnp.float32)

    L = DEPTH
    u = jax.random.uniform(ks[19], (L, LRU_WIDTH), jnp.float32, minval=0.9, maxval=0.999)
    a0 = u ** (1.0 / LRU_C)
    lru_lambda = jnp.log(a0) - jnp.log1p(-a0)
    return {
        "x": nrm(ks[0], (BATCH, SEQ, D_MODEL), 1.0),
        "c": nrm(ks[1], (BATCH, D_MODEL), 1.0),
        "ada_w": nrm(ks[2], (L, D_MODEL, 6 * D_MODEL), D_MODEL ** -0.5),
        "ada_b": nrm(ks[3], (L, 6 * D_MODEL), 0.01),
        "norm1_g": 1.0 + nrm(ks[4], (L, D_MODEL), 0.02),
        "w_in": nrm(ks[5], (L, D_MODEL, D_IN_PROJ), D_MODEL ** -0.5),
        "q_norm_g": 1.0 + nrm(ks[6], (L, ATTN_DK), 0.02),
        "k_norm_g": 1.0 + nrm(ks[7], (L, ATTN_DK), 0.02),
        "lambda_q1": nrm(ks[8], (L, ATTN_DK), 0.1),
        "lambda_k1": nrm(ks[9], (L, ATTN_DK), 0.1),
        "lambda_q2": nrm(ks[10], (L, ATTN_DK), 0.1),
        "lambda_k2": nrm(ks[11], (L, ATTN_DK), 0.1),
        "attn_subln_g": 1.0 + nrm(ks[12], (L, ATTN_DV), 0.02),
        "conv_w": nrm(ks[13], (L, CONV_W, LRU_WIDTH), CONV_W ** -0.5),
        "conv_b": nrm(ks[14], (L, LRU_WIDTH), 0.01),
        "lru_wa": nrm(ks[15], (L, LRU_BLOCKS, LRU_BW, LRU_BW), LRU_BW ** -0.5),
        "lru_ba": nrm(ks[16], (L, LRU_WIDTH), 0.01),
        "lru_wx": nrm(ks[17], (L, LRU_BLOCKS, LRU_BW, LRU_BW), LRU_BW ** -0.5),
        "lru_bx": nrm(ks[18], (L, LRU_WIDTH), 0.01),
        "lru_lambda": lru_lambda,
        "lru_out_g": 1.0 + nrm(ks[20], (L, LRU_WIDTH), 0.02),
        "w_out": nrm(ks[21], (L, D_MODEL, D_MODEL), D_MODEL ** -0.5),
        "norm2_g": 1.0 + nrm(ks[22], (L, D_MODEL), 0.02),
        "router_w": nrm(ks[23], (L, D_MODEL, N_EXPERTS), D_MODEL ** -0.5),
        "router_b": nrm(ks[24], (L, N_EXPERTS), 0.01),
        "w_gate_up": nrm(ks[25], (L, N_EXPERTS, D_MODEL, 2 * D_FF), D_MODEL ** -0.5),
        "b_gate_up": nrm(ks[26], (L, N_EXPERTS, 2 * D_FF), 0.01),
        "w_down": nrm(ks[27], (L, N_EXPERTS, D_FF, D_MODEL), D_FF ** -0.5),
        "b_down": nrm(ks[28], (L, N_EXPERTS, D_MODEL), 0.01),
    }


def reference(x, c, ada_w, ada_b, norm1_g, w_in, q_norm_g, k_norm_g,
              lambda_q1, lambda_k1, lambda_q2, lambda_k2, attn_subln_g,
              conv_w, conv_b, lru_wa, lru_ba, lru_wx, lru_bx, lru_lambda, lru_out_g,
              w_out, norm2_g, router_w, router_b, w_gate_up, b_gate_up, w_down, b_down):
    B, S, D = x.shape
    for l in range(DEPTH):
        lam_init = 0.8 - 0.6 * math.exp(-0.3 * l)
        mod = jax.nn.silu(c) @ ada_w[l] + ada_b[l]
        sh1, sc1, g1, sh2, sc2, g2 = jnp.split(mod, 6, axis=-1)

        h = modulate(rms_norm(x, norm1_g[l]), sh1, sc1)
        proj = h @ w_in[l]
        q, k, v, xr, gr = jnp.split(
            proj, [QK_WIDTH, 2 * QK_WIDTH, 2 * QK_WIDTH + ATTN_WIDTH,
                   2 * QK_WIDTH + ATTN_WIDTH + LRU_WIDTH], axis=-1)
        q = rms_norm(q.reshape(B, S, ATTN_HEADS, 2, ATTN_DK), q_norm_g[l])
        k = rms_norm(k.reshape(B, S, ATTN_HEADS, 2, ATTN_DK), k_norm_g[l])
        v = v.reshape(B, S, ATTN_HEADS, ATTN_DV)
        lam = (jnp.exp(jnp.sum(lambda_q1[l].astype(jnp.float32) * lambda_k1[l].astype(jnp.float32)))
               - jnp.exp(jnp.sum(lambda_q2[l].astype(jnp.float32) * lambda_k2[l].astype(jnp.float32)))
               + lam_init)
        attn = causal_diff_attention(q, k, v, lam)
        attn = (rms_norm(attn, attn_subln_g[l]) * (1.0 - lam_init)).reshape(B, S, ATTN_WIDTH)
        lru = rg_lru_branch(xr, gr, conv_w[l], conv_b[l], lru_wa[l], lru_ba[l],
                            lru_wx[l], lru_bx[l], lru_lambda[l])
        lru = rms_norm(lru, lru_out_g[l])
        mix = jnp.concatenate([attn, lru], axis=-1) @ w_out[l]
        x = x + g1[:, None, :] * mix

        h = modulate(rms_norm(x, norm2_g[l]), sh2, sc2)
        y = moe_ffn(h, router_w[l], router_b[l], w_gate_up[l], b_gate_up[l], w_down[l], b_down[l])
        x = x + g2[:, None, :] * y
    return x
```

```python
import math
from contextlib import ExitStack

import numpy as np
import concourse.bass as bass
import concourse.mybir as mybir
from concourse.bass_utils import run_bass_kernel_spmd

F32 = mybir.dt.float32
BF16 = mybir.dt.bfloat16
I32 = mybir.dt.int32
U32 = mybir.dt.uint32
AF = mybir.ActivationFunctionType
ALU = mybir.AluOpType
AX = mybir.AxisListType

NCORES = 8
D = 1024
S = 4096
NB = 2
T = NB * S
NTILE = T // 128
QB = 512
NBLK = S // QB
H = 4
DK = 64
DV = 128
LRU_C = 4
NE = 32
TOPK = 4
DFF = 1024
MB = 256
NSLOT = T * TOPK
NMB = NSLOT // MB + NE
PSLOT = NMB * MB
EPS = 1e-6
LAM_INIT = 0.8 - 0.6 * math.exp(-0.3 * 0)
SAME_ENGINE_SYNC = True


class Tok:
    __slots__ = ("sem", "val", "eng", "key")

    def __init__(self, sem, val, eng, key):
        self.sem, self.val, self.eng, self.key = sem, val, eng, key


class Res:
    def __init__(self, name, track_waw=True):
        self.name = name
        self.w = None
        self.r = {}
        self.track_waw = track_waw


class Tl(Res):
    def __init__(self, name, ap, track_waw=True):
        super().__init__(name, track_waw)
        self.ap = ap

    def __getitem__(self, idx):
        return self.ap[idx]


class Alias(Tl):
    def __init__(self, parent, ap):
        self.parent = parent
        self.name = parent.name
        self.ap = ap
        self.track_waw = parent.track_waw

    @property
    def w(self):
        return self.parent.w

    @w.setter
    def w(self, v):
        self.parent.w = v

    @property
    def r(self):
        return self.parent.r

    @r.setter
    def r(self, v):
        self.parent.r = v


def alias(parent, shape, dtype, off_bytes=0):
    pdt_size = {F32: 4, BF16: 2, I32: 4, U32: 4}
    flat = parent.ap
    if len(flat.shape) > 2:
        names = " ".join("d%d" % i for i in range(len(flat.shape) - 1))
        flat = flat.rearrange("p %s -> p (%s)" % (names, names))
    psz = pdt_size[parent.ap.dtype]
    n = int(np.prod(shape))
    nbytes = n * pdt_size[dtype]
    a = flat[:, off_bytes // psz:(off_bytes + nbytes) // psz]
    if dtype != parent.ap.dtype:
        a = a.bitcast(dtype)
    if len(shape) > 1:
        names = " ".join("d%d" % i for i in range(len(shape)))
        kw = {"d%d" % i: int(shape[i]) for i in range(1, len(shape))}
        a = a.rearrange("p (%s) -> p %s" % (names, names), **kw)
    return Alias(parent, a)


class KB:
    def __init__(self, nc):
        self.nc = nc
        self.E = {}
        for name, eng in (("pe", nc.tensor), ("act", nc.scalar), ("dve", nc.vector),
                          ("pool", nc.gpsimd), ("sp", nc.sync)):
            self.E[name] = dict(eng=eng, sem=nc.alloc_semaphore("S_" + name), seq=0,
                                known={}, ptok=None)
        self.dsem = {}
        self.uid = 0

    def sb(self, name, shape, dtype, **kw):
        return Tl(name, self.nc.alloc_sbuf_tensor(name, list(shape), dtype).ap(), **kw)

    def ps(self, name, shape, dtype=F32):
        return Tl(name, self.nc.alloc_psum_tensor(name, list(shape), dtype).ap())

    def dram(self, name, shape, dtype, kind="Internal", **kw):
        return Tl(name, self.nc.dram_tensor(name, list(shape), dtype, kind=kind).ap(), **kw)

    def _deps(self, reads, writes):
        toks = []
        for r in reads:
            toks.append(r.w)
        for w in writes:
            if w.track_waw:
                toks.append(w.w)
                toks.extend(w.r.values())
        return toks

    def _wait_for(self, en, toks):
        E = self.E[en]
        need = {}
        for t in toks:
            if t is None:
                continue
            if t.eng == en and (en == "pe" or not SAME_ENGINE_SYNC):
                continue
            val = t.val
            if t.eng is None:
                val = self.dsem[t.key][1]
            elif val is None:
                if t.eng == en:
                    continue
                raise RuntimeError("dependency on unsignalled op of %s" % t.eng)
            if val > E["known"].get(t.key, 0) and val > need.get(t.key, (None, 0))[1]:
                need[t.key] = (t.sem, val)
        for key, (sem, val) in need.items():
            E["eng"].wait_ge(sem, val)
            E["known"][key] = val

    @staticmethod
    def _commit(tok, reads, writes):
        for r in reads:
            r.r[tok.key] = tok
        for w in writes:
            w.w = tok
            w.r = {}

    def op(self, en, fn, reads=(), writes=(), signal=True):
        E = self.E[en]
        self._wait_for(en, self._deps(reads, writes))
        inst = fn(E["eng"])
        if E["ptok"] is None:
            E["ptok"] = Tok(E["sem"], None, en, "S_" + en)
        tok = E["ptok"]
        self._commit(tok, reads, writes)
        if signal:
            E["seq"] += 1
            inst.then_inc(E["sem"], 1)
            tok.val = E["seq"]
            E["ptok"] = None
        return inst

    def dma(self, q, out, in_, reads=(), writes=(), sem=None, indirect=None, join=False, **kw):
        E = self.E[q]
        self._wait_for(q, self._deps(reads, writes))
        if sem not in self.dsem:
            self.dsem[sem] = [self.nc.alloc_semaphore("D_" + sem), 0]
        s = self.dsem[sem]
        if not join and s[1] > E["known"].get(sem, 0):
            E["eng"].wait_ge(s[0], s[1])
            E["known"][sem] = s[1]
        s[1] += 16
        if indirect is None:
            inst = E["eng"].dma_start(out=out, in_=in_, **kw)
        else:
            inst = E["eng"].indirect_dma_start(out=out, in_=in_, **indirect, **kw)
        inst.then_inc(s[0], 16)
        tok = Tok(s[0], s[1], None, sem)
        self._commit(tok, reads, writes)
        return inst

    def finish(self):
        sp = self.E["sp"]["eng"]
        for name, (sem, cnt) in self.dsem.items():
            if cnt > self.E["sp"]["known"].get(name, 0):
                sp.wait_ge(sem, cnt)
        for en in ("pe", "act", "dve", "pool"):
            E = self.E[en]
            if E["seq"] > 0:
                sp.wait_ge(E["sem"], E["seq"])


def build_program(stop_after=None):
    nc = bass.Bass("TRN2", target_bir_lowering=False)
    k = KB(nc)

    INSHAPES = {
        "x": [T, D], "cT": [128, 8, NB], "ada_w": [D, 6 * D], "ada_bT": [128, 48], "ada_b": [1, 6 * D],
        "n1g_col": [128, 8], "n2g_row": [1, D], "w_in": [D, 2560], "qg_col": [128, 1], "kg_col": [128, 1],
        "lamv": [1, 4 * DK], "subg_row": [1, DV], "conv_col": [128, LRU_C, 5], "lru_blk": [128, 2, LRU_C, 128],
        "lru_vec": [128, LRU_C, 4], "w_out": [D, D], "router_w": [D, NE], "router_b": [1, NE],
        "w_gu": [NE, D, 2 * DFF], "b_gu_col": [NE, 128, 16], "w_dn": [NE, DFF, D], "b_dn": [NE, 1, D],
    }
    used_inputs = {}

    def din(name):
        if name not in used_inputs:
            used_inputs[name] = Tl(name, nc.dram_tensor(name, list(INSHAPES[name]), F32, kind="ExternalInput").ap())
        return used_inputs[name]

    nc._used_inputs = used_inputs
    x_in = din("x")
    out_t = Tl("out", nc.dram_tensor("out", [T, D], F32, kind="ExternalOutput").ap(), track_waw=False)

    x1_d = k.dram("x1_d", [T, D], F32, track_waw=False)

    ident = k.sb("ident", [128, 128], BF16)
    tri = k.sb("tri", [128, 128], BF16)
    ones_bf = k.sb("ones_bf", [128, 128], BF16)
    iot = k.sb("iot", [128, 128], F32)
    k.op("pool", lambda e: e.iota(iot.ap, [[1, 128]], base=0, channel_multiplier=-1,
                                  allow_small_or_imprecise_dtypes=True), writes=[iot])
    k.op("dve", lambda e: e.tensor_scalar(out=ident.ap, in0=iot.ap, scalar1=0.0, scalar2=None,
                                          op0=ALU.is_equal), reads=[iot], writes=[ident])
    k.op("dve", lambda e: e.tensor_scalar(out=tri.ap, in0=iot.ap, scalar1=0.0, scalar2=None,
                                          op0=ALU.is_ge), reads=[iot], writes=[tri])
    k.op("dve", lambda e: e.memset(ones_bf.ap, 1.0), writes=[ones_bf])
    negtri = k.sb("negtri", [128, 128], BF16)
    k.op("dve", lambda e: e.tensor_scalar(out=negtri.ap, in0=iot.ap, scalar1=0.0, scalar2=-30000.0,
                                          op0=ALU.is_lt, op1=ALU.mult), reads=[iot], writes=[negtri])
    epst = k.sb("epst", [128, 2], F32)
    k.op("dve", lambda e: e.memset(epst[:, 0:1], float(EPS)), writes=[epst])
    k.op("dve", lambda e: e.memset(epst[:, 1:2], 1.0), writes=[epst])

    def rstd_act(dst_ap, src_ap, res_list, scale):
        k.op("act", lambda e: e.activation(out=dst_ap, in_=src_ap, func=AF.Ln, scale=float(scale), bias=epst[:, 0:1]),
             reads=res_list + [epst], writes=res_list)
        k.op("act", lambda e: e.activation(out=dst_ap, in_=dst_ap, func=AF.Exp, scale=-0.5), reads=res_list, writes=res_list)

    psall = nc.alloc_psum_tensor("psall", [128, 8 * 512], F32).ap()
    pbank = [Tl("pb%d" % i, psall[:, i * 512:(i + 1) * 512]) for i in range(8)]

    def bank_bf(b):
        return b.ap.bitcast(BF16)

    modcol = k.sb("modcol", [128, 6, 8, NB], F32)
    A1 = k.sb("A1", [128, NB, 8], F32)
    B1 = k.sb("B1", [128, NB, 8], F32)
    lam_col = k.sb("lam_col", [128, 1], F32)
    nlam_col = k.sb("nlam_col", [128, 1], F32)

    idxw = k.sb("idxw", [128, NMB], I32)
    idxb = k.sb("idxb", [128, NMB], I32)
    idxs = k.sb("idxs", [128, NMB], I32)
    es_w = ExitStack()
    w_in_sb = Tl("w_in_sb", es_w.enter_context(nc.sbuf_tensor("w_in_sb", [128, 8, 2560], BF16)).ap())
    w_out_sb = Tl("w_out_sb", es_w.enter_context(nc.sbuf_tensor("w_out_sb", [128, 8, D], BF16)).ap())
    w_in_v = din("w_in").ap.rearrange("(k p) f -> p k f", p=128)
    for kk in range(8):
        k.dma("pool", w_in_sb[:, kk, :], w_in_v[:, kk, :], writes=[w_in_sb], sem="w_in", max_dma_last_dim=4096, join=(kk > 0))
    w_out_v = din("w_out").ap.rearrange("(k p) f -> p k f", p=128)
    for kk in range(8):
        k.dma("pool", w_out_sb[:, kk, :], w_out_v[:, kk, :], writes=[w_out_sb], sem="w_out", max_dma_last_dim=4096, join=(kk > 0))

    modrow_d = k.dram("modrow_d", [6, NB, 128, D], F32, track_waw=False)
    with ExitStack() as es0:
        def sb0(name, shape, dtype):
            return Tl(name, es0.enter_context(nc.sbuf_tensor(name, list(shape), dtype)).ap())

        cT = sb0("cT_sb", [128, 8, NB], F32)
        scb = sb0("scb", [128, 8, NB], BF16)
        sc_rep = [sb0("sc_rep%d" % b, [128, 8, 128], BF16) for b in range(NB)]
        wsec = [sb0("wsec%d" % i, [128, 8, 1024], BF16) for i in range(2)]
        bT = sb0("bT", [128, 48], F32)
        brow = [sb0("brow%d" % i, [128, 1024], F32) for i in range(2)]
        n1g = sb0("n1g", [128, 8], F32)
        rowtmp = [sb0("rowtmp%d" % b, [128, D], F32) for b in range(2)]
        lv = sb0("lv", [128, 4 * DK], F32)
        lvp = sb0("lvp", [128, 2 * DK], F32)
        lsum = sb0("lsum", [128, 2], F32)

        k.dma("sp", cT.ap, din("cT").ap, writes=[cT], sem="c0")
        k.dma("sp", bT.ap, din("ada_bT").ap, writes=[bT], sem="c1")
        k.dma("sp", n1g.ap, din("n1g_col").ap, writes=[n1g], sem="c2")
        k.dma("sp", lv.ap, din("lamv").ap.to_broadcast([128, 4 * DK]), writes=[lv], sem="c3")
        k.op("act", lambda e: e.activation(out=scb.ap, in_=cT.ap, func=AF.Silu), reads=[cT], writes=[scb])
        for b in range(NB):
            k.op("dve", lambda e, b=b: e.tensor_copy(out=sc_rep[b].ap, in_=scb[:, :, b:b + 1].to_broadcast([128, 8, 128])),
                 reads=[scb], writes=[sc_rep[b]])
        k.op("dve", lambda e: e.tensor_tensor(out=lvp.ap.rearrange("p (a d) -> p a d", a=2),
                                              in0=lv.ap.rearrange("p (a t d) -> p a t d", a=2, t=2)[:, :, 0, :],
                                              in1=lv.ap.rearrange("p (a t d) -> p a t d", a=2, t=2)[:, :, 1, :],
                                              op=ALU.mult), reads=[lv], writes=[lvp])
        k.op("dve", lambda e: e.tensor_reduce(out=lsum.ap, in_=lvp.ap.rearrange("p (a d) -> p a d", a=2),
                                              axis=AX.X, op=ALU.add), reads=[lvp], writes=[lsum])
        k.op("act", lambda e: e.activation(out=lsum.ap, in_=lsum.ap, func=AF.Exp), reads=[lsum], writes=[lsum])
        k.op("dve", lambda e: e.tensor_tensor(out=lam_col.ap, in0=lsum[:, 0:1], in1=lsum[:, 1:2], op=ALU.subtract),
             reads=[lsum], writes=[lam_col])
        k.op("dve", lambda e: e.tensor_scalar(out=lam_col.ap, in0=lam_col.ap, scalar1=float(LAM_INIT), scalar2=None,
                                              op0=ALU.add), reads=[lam_col], writes=[lam_col])
        k.op("dve", lambda e: e.tensor_scalar(out=nlam_col.ap, in0=lam_col.ap, scalar1=-1.0, scalar2=None,
                                              op0=ALU.mult), reads=[lam_col], writes=[nlam_col])

        ada_v = din("ada_w").ap.rearrange("(k p) f -> p k f", p=128)
        for sec in range(6):
            wb = wsec[sec % 2]
            k.dma("pool", wb.ap, ada_v[:, :, sec * 1024:(sec + 1) * 1024], writes=[wb], sem="wsec%d" % (sec % 2))
            if sec in (0, 1):
                pb = pbank[5 + sec % 2]
                pv = pb.ap[:, 0:16].rearrange("p (j b) -> p j b", b=NB)
                for j in range(8):
                    for kk in range(8):
                        first = (j == 0 and kk == 0)
                        last = (j == 7 and kk == 7)
                        k.op("pe", lambda e, j=j, kk=kk, first=first, last=last: e.matmul(
                            pv[:, j, :], lhsT=wb[:, kk, j * 128:(j + 1) * 128], rhs=scb[:, kk, :],
                            start=first, stop=(kk == 7), skip_group_check=True),
                            reads=[wb, scb], writes=[pb], signal=last)
                k.op("dve", lambda e, sec=sec, pv=pv: e.tensor_tensor(
                    out=modcol[:, sec, :, :], in0=pv,
                    in1=bT[:, sec * 8:(sec + 1) * 8].unsqueeze(2).to_broadcast([128, 8, NB]), op=ALU.add),
                    reads=[pb, bT], writes=[modcol])
            else:
                br = brow[sec % 2]
                k.dma("sp", br.ap, din("ada_b").ap[:, sec * 1024:(sec + 1) * 1024].to_broadcast([128, 1024]), writes=[br], sem="brow")
                for b in range(NB):
                    dst = rowtmp[b]
                    for half in range(2):
                        pb = pbank[5 + (b * 2 + half) % 3]
                        for kk in range(8):
                            k.op("pe", lambda e, kk=kk, b=b, half=half, pb=pb: e.matmul(
                                pb.ap, lhsT=sc_rep[b][:, kk, :], rhs=wb[:, kk, half * 512:(half + 1) * 512],
                                start=(kk == 0), stop=(kk == 7)),
                                reads=[wb, sc_rep[b]], writes=[pb], signal=(kk == 7))
                        k.op("dve", lambda e, dst=dst, half=half, pb=pb, br=br: e.tensor_tensor(
                            out=dst[:, half * 512:(half + 1) * 512], in0=pb.ap, in1=br[:, half * 512:(half + 1) * 512],
                            op=ALU.add), reads=[pb, br], writes=[dst])
                    k.dma("sp", modrow_d.ap[sec, b], dst.ap, reads=[dst], writes=[modrow_d], sem="rowst%d" % b)
        for b in range(NB):
            k.op("dve", lambda e, b=b: e.scalar_tensor_tensor(out=A1[:, b, :], in0=modcol[:, 1, :, b], scalar=1.0,
                                                              in1=n1g.ap, op0=ALU.add, op1=ALU.mult),
                 reads=[modcol, n1g], writes=[A1])
            k.op("dve", lambda e, b=b: e.tensor_copy(out=B1[:, b, :], in_=modcol[:, 0, :, b]),
                 reads=[modcol], writes=[B1])
        k_all_quiesce(k)

    if stop_after == "p0":
        dbg = k.sb("dbg", [128, D], F32)
        k.op("dve", lambda e: e.memset(dbg.ap, 0.0), writes=[dbg])
        k.op("dve", lambda e: e.tensor_copy(out=dbg[:, 0:16], in_=A1.ap.rearrange("p b k -> p (b k)")), reads=[A1], writes=[dbg])
        k.op("dve", lambda e: e.tensor_copy(out=dbg[:, 16:32], in_=B1.ap.rearrange("p b k -> p (b k)")), reads=[B1], writes=[dbg])
        k.op("dve", lambda e: e.tensor_copy(out=dbg[:, 32:33], in_=lam_col.ap), reads=[lam_col], writes=[dbg])
        k.dma("sp", out_t.ap[0:128, :], dbg.ap, reads=[dbg], sem="ost")
        for b in range(NB):
            k.dma("sp", dbg.ap, modrow_d.ap[2, b], reads=[modrow_d], writes=[dbg], sem="dbgl")
            k.dma("sp", out_t.ap[128 * (b + 1):128 * (b + 2), :], dbg.ap, reads=[dbg], sem="ost")
        k.finish()
        return nc

    full = stop_after is None
    xs_d = k.dram("xs_d", [PSLOT, D], BF16, track_waw=False)
    xs_init = Res("xs_init")
    wgu_bf_d = k.dram("wgu_bf_d", [NE * 128, 8 * 2 * DFF], BF16, track_waw=False)
    wdn_bf_d = k.dram("wdn_bf_d", [NE * 128, 8 * D], BF16, track_waw=False)

    def convert_expert(e, first, after=()):
        k.dma("pool", wgu_bf_d.ap[e * 128:(e + 1) * 128, :].rearrange("p (k f) -> p k f", k=8),
              din("w_gu").ap[e].rearrange("(k p) f -> p k f", p=128), reads=list(after), writes=[wgu_bf_d], sem="wconv", join=not first)
        k.dma("pool", wdn_bf_d.ap[e * 128:(e + 1) * 128, :].rearrange("p (k f) -> p k f", k=8),
              din("w_dn").ap[e].rearrange("(k p) f -> p k f", p=128), reads=list(after), writes=[wdn_bf_d], sem="wconv", join=True)

    x1_dst = out_t if stop_after == "p1" else x1_d
    with ExitStack() as es1:
        def sb1(name, shape, dtype):
            return Tl(name, es1.enter_context(nc.sbuf_tensor(name, list(shape), dtype)).ap())

        kT_blk = [sb1("kT%d" % i, [128, H, QB], BF16) for i in range(NBLK)]
        V_blk = [sb1("V%d" % i, [128, 4, H, 130], BF16) for i in range(NBLK)]
        xb = [sb1("xb%d" % i, [128, D], F32) for i in range(2)]
        arenaA = sb1("arenaA", [128, 2048], F32)
        xn = alias(arenaA, [4, D], BF16)
        arenaB = sb1("arenaB", [128, 2048], F32)
        hT = alias(arenaB, [8, QB], BF16)
        attnT = alias(arenaB, [4, QB], BF16)
        lru_nb = alias(arenaB, [LRU_C, QB], BF16, off_bytes=4096)
        arenaC = sb1("arenaC", [128, 512], F32)
        junk = alias(arenaC, [D], BF16)
        n_sq = alias(arenaC, [512], F32)
        t_gg = alias(arenaC, [QB], F32)
        qT = sb1("qT", [128, 2, H, QB], BF16)
        Et = [sb1("E%d" % i, [128, 2, QB], BF16) for i in range(2)]
        attn_n = sb1("attn_n", [128, 4, 512], BF16)
        t_mix = alias(attn_n, [D], F32)
        xr_ext = sb1("xr_ext", [128, LRU_C, QB + 4], F32)
        lru_f = sb1("lru_f", [128, LRU_C, QB], BF16)
        blk_f = alias(lru_f, [2, LRU_C, 128], F32)
        x1b = [alias(lru_f, [D], F32)]
        e_o1 = sb1("e_o1", [128, 3, 128], F32)
        e_sq = sb1("e_sq", [128, 3, 128], F32)
        hstate = sb1("hstate", [128, LRU_C], F32)
        t_xc = sb1("t_xc", [128, QB], F32)
        t_xcb = sb1("t_xcb", [128, QB], BF16)
        t_sqb = alias(t_xcb, [QB], BF16)
        t_r = sb1("t_r", [128, QB], F32)
        t_rl = alias(t_r, [QB], F32)
        t_i = sb1("t_i", [128, QB], F32)
        t_a = sb1("t_a", [128, QB], F32)
        t_a2 = sb1("t_a2", [128, QB], F32)
        t_h = sb1("t_h", [128, QB], F32)
        e_t2 = alias(t_h, [3, 128], F32)
        t_sq = sb1("t_sq", [128, QB], F32)
        n_qn_l2 = [[sb1("n_qn%d_%d" % (p, g), [128, 512], BF16) for g in range(2)] for p in range(2)]
        st8_l2 = [[sb1("st8_%d_%d" % (p, g), [128, 8], F32) for g in range(2)] for p in range(2)]
        e_st = sb1("e_st", [128, 12], F32)
        stat = sb1("stat", [128, 16], F32)
        cexp = sb1("cexp", [128, 4], F32)
        qg = sb1("qg", [128, 1], F32)
        kg = sb1("kg", [128, 1], F32)
        subg_bc = sb1("subg_bc", [128, DV], F32)
        convc = sb1("convc", [128, LRU_C, 5], F32)
        blk_b = sb1("blk_b", [128, 2, LRU_C, 128], BF16)
        lvec = sb1("lvec", [128, LRU_C, 4], F32)
        coef = sb1("coef", [128, LRU_C, 2], F32)
        g1_cur = sb1("g1_cur", [128, D], F32)

        k.dma("sp", qg.ap, din("qg_col").ap, writes=[qg], sem="c0")
        k.dma("sp", kg.ap, din("kg_col").ap, writes=[kg], sem="c1")
        k.dma("sp", subg_bc.ap, din("subg_row").ap.to_broadcast([128, DV]), writes=[subg_bc], sem="c2")
        k.dma("sp", convc.ap, din("conv_col").ap, writes=[convc], sem="c3")
        k.dma("sp", blk_f.ap, din("lru_blk").ap, writes=[blk_f], sem="c0")
        k.dma("sp", lvec.ap, din("lru_vec").ap, writes=[lvec], sem="c1")
        k.op("dve", lambda e: e.tensor_copy(out=blk_b.ap, in_=blk_f.ap), reads=[blk_f], writes=[blk_b])
        k.op("dve", lambda e: e.tensor_scalar(out=qg.ap, in0=qg.ap, scalar1=float(DK ** -0.5), scalar2=None, op0=ALU.mult),
             reads=[qg], writes=[qg])
        k.op("dve", lambda e: e.tensor_scalar(out=subg_bc.ap, in0=subg_bc.ap, scalar1=float(1.0 - LAM_INIT), scalar2=None,
                                              op0=ALU.mult), reads=[subg_bc], writes=[subg_bc])
        k.op("dve", lambda e: e.memset(cexp[:, 0:1], -0.5), writes=[cexp])
        k.op("dve", lambda e: e.memset(cexp[:, 1:2], 0.5), writes=[cexp])
        k.op("act", lambda e: e.activation(out=coef[:, :, 0], in_=lvec[:, :, 2], func=AF.Exp, scale=-1.0),
             reads=[lvec], writes=[coef])
        k.op("dve", lambda e: e.tensor_scalar(out=coef[:, :, 0], in0=coef[:, :, 0], scalar1=1.0, scalar2=None, op0=ALU.add),
             reads=[coef], writes=[coef])
        k.op("act", lambda e: e.activation(out=coef[:, :, 0], in_=coef[:, :, 0], func=AF.Ln), reads=[coef], writes=[coef])
        k.op("dve", lambda e: e.tensor_scalar(out=coef[:, :, 1], in0=coef[:, :, 0], scalar1=-16.0, scalar2=None, op0=ALU.mult),
             reads=[coef], writes=[coef])
        k.op("dve", lambda e: e.tensor_scalar(out=coef[:, :, 0], in0=coef[:, :, 0], scalar1=-8.0, scalar2=None, op0=ALU.mult),
             reads=[coef], writes=[coef])
        for i in range(NBLK):
            k.op("pool", lambda e, i=i: e.memset(V_blk[i][:, :, :, 128:130], 1.0), writes=[V_blk[i]])
        k.op("pool", lambda e: e.memset(qT.ap, 0.0), writes=[qT])

        gstate = {"n": 0, "banks": [7]}

        def gbank():
            bl = gstate["banks"]
            b = pbank[bl[gstate["n"] % len(bl)]]
            gstate["n"] += 1
            return b

        def rstd_pow(dst_ap, src_ap, res_list, scale, n):
            rstd_act(dst_ap, src_ap, res_list, scale)

        def acc_ap(c, s):
            if s < 3:
                return pbank[4 + c].ap[:, s * 129:(s + 1) * 129]
            return pbank[6].ap[:, c * 129:(c + 1) * 129]

        def acc_bank(c, s):
            return pbank[4 + c] if s < 3 else pbank[6]

        def lru_stage_a(b, i, c):
            ps_x = gbank()
            for kk in range(8):
                k.op("pe", lambda e, kk=kk: e.matmul(ps_x.ap, lhsT=w_in_sb[:, kk, 1536 + c * 128:1536 + (c + 1) * 128],
                                                    rhs=hT[:, kk, :], start=(kk == 0), stop=(kk == 7)),
                     reads=[w_in_sb, hT], writes=[ps_x], signal=(kk == 7))
            k.op("dve", lambda e: e.tensor_copy(out=xr_ext[:, c, 3:3 + QB], in_=ps_x.ap), reads=[ps_x], writes=[xr_ext])
            k.op("dve", lambda e: e.tensor_scalar(out=t_xc.ap, in0=xr_ext[:, c, 0:QB], scalar1=convc[:, c, 0:1],
                                                  scalar2=convc[:, c, 4:5], op0=ALU.mult, op1=ALU.add),
                 reads=[xr_ext, convc], writes=[t_xc])
            for j in range(1, 4):
                k.op("dve", lambda e, j=j: e.scalar_tensor_tensor(out=t_xc.ap, in0=xr_ext[:, c, j:j + QB],
                                                                 scalar=convc[:, c, j:j + 1], in1=t_xc.ap,
                                                                 op0=ALU.mult, op1=ALU.add),
                     reads=[xr_ext, convc, t_xc], writes=[t_xc])
            k.op("dve", lambda e: e.tensor_copy(out=xr_ext[:, c, 0:3], in_=xr_ext[:, c, QB:QB + 3]),
                 reads=[xr_ext], writes=[xr_ext])
            k.op("dve", lambda e: e.tensor_copy(out=t_xcb.ap, in_=t_xc.ap), reads=[t_xc], writes=[t_xcb])

        def lru_stage_b(b, i, c):
            ps_g = gbank()
            for kk in range(8):
                k.op("pe", lambda e, kk=kk: e.matmul(ps_g.ap, lhsT=w_in_sb[:, kk, 2048 + c * 128:2048 + (c + 1) * 128],
                                                    rhs=hT[:, kk, :], start=(kk == 0), stop=(kk == 7)),
                     reads=[w_in_sb, hT], writes=[ps_g], signal=(kk == 7))
            k.op("dve", lambda e: e.tensor_copy(out=t_gg.ap, in_=ps_g.ap), reads=[ps_g], writes=[t_gg])
            k.op("dve", lambda e: e.tensor_tensor(out=t_a2.ap, in0=t_gg.ap, in1=t_gg.ap, op=ALU.mult), reads=[t_gg], writes=[t_a2])
            k.op("dve", lambda e: e.tensor_scalar(out=t_a2.ap, in0=t_a2.ap, scalar1=0.044715, scalar2=1.0, op0=ALU.mult, op1=ALU.add),
                 reads=[t_a2], writes=[t_a2])
            k.op("dve", lambda e: e.tensor_tensor(out=t_a2.ap, in0=t_a2.ap, in1=t_gg.ap, op=ALU.mult), reads=[t_a2, t_gg], writes=[t_a2])
            k.op("act", lambda e: e.activation(out=t_a2.ap, in_=t_a2.ap, func=AF.Sigmoid, scale=1.5957691216057308),
                 reads=[t_a2], writes=[t_a2])
            k.op("dve", lambda e: e.tensor_tensor(out=t_gg.ap, in0=t_gg.ap, in1=t_a2.ap, op=ALU.mult), reads=[t_gg, t_a2], writes=[t_gg])
            ps_r = gbank()
            k.op("pe", lambda e: e.matmul(ps_r.ap, lhsT=blk_b[:, 0, c, :], rhs=t_xcb.ap, start=True, stop=True),
                 reads=[blk_b, t_xcb], writes=[ps_r])
            k.op("act", lambda e: e.activation(out=t_r.ap, in_=ps_r.ap, func=AF.Sigmoid, bias=lvec[:, c, 0:1]),
                 reads=[ps_r, lvec], writes=[t_r])
            ps_i = gbank()
            k.op("pe", lambda e: e.matmul(ps_i.ap, lhsT=blk_b[:, 1, c, :], rhs=t_xcb.ap, start=True, stop=True),
                 reads=[blk_b, t_xcb], writes=[ps_i])
            k.op("act", lambda e: e.activation(out=t_i.ap, in_=ps_i.ap, func=AF.Sigmoid, bias=lvec[:, c, 1:2]),
                 reads=[ps_i, lvec], writes=[t_i])
            k.op("act", lambda e: e.activation(out=t_a.ap, in_=t_r.ap, func=AF.Exp, scale=coef[:, c, 0:1]),
                 reads=[t_r, coef], writes=[t_a])
            k.op("dve", lambda e: e.tensor_tensor(out=t_a2.ap, in0=t_a.ap, in1=t_a.ap, op=ALU.mult), reads=[t_a], writes=[t_a2])
            k.op("act", lambda e: e.activation(out=t_a2.ap, in_=t_a2.ap, func=AF.Ln, scale=-1.0, bias=epst[:, 1:2]),
                 reads=[t_a2, epst], writes=[t_a2])
            k.op("act", lambda e: e.activation(out=t_a2.ap, in_=t_a2.ap, func=AF.Exp, scale=0.5), reads=[t_a2], writes=[t_a2])
            k.op("dve", lambda e: e.tensor_tensor(out=t_i.ap, in0=t_i.ap, in1=t_xc.ap, op=ALU.mult),
                 reads=[t_i, t_xc], writes=[t_i])
            k.op("dve", lambda e: e.tensor_tensor(out=t_i.ap, in0=t_i.ap, in1=t_a2.ap, op=ALU.mult),
                 reads=[t_i, t_a2], writes=[t_i])
            k.op("dve", lambda e: e.tensor_tensor_scan(out=t_h.ap, data0=t_a.ap, data1=t_i.ap, initial=hstate[:, c:c + 1],
                                                       op0=ALU.mult, op1=ALU.add),
                 reads=[t_a, t_i, hstate], writes=[t_h])
            k.op("dve", lambda e: e.tensor_copy(out=hstate[:, c:c + 1], in_=t_h[:, QB - 1:QB]), reads=[t_h], writes=[hstate])
            k.op("dve", lambda e: e.tensor_tensor(out=lru_f[:, c, :], in0=t_h.ap, in1=t_gg.ap, op=ALU.mult),
                 reads=[t_h, t_gg], writes=[lru_f])
            if c == 0:
                k.op("dve", lambda e: e.tensor_tensor(out=t_sq.ap, in0=lru_f[:, c, :], in1=lru_f[:, c, :], op=ALU.mult),
                     reads=[lru_f], writes=[t_sq])
            else:
                k.op("dve", lambda e: e.tensor_tensor(out=t_rl.ap, in0=lru_f[:, c, :], in1=lru_f[:, c, :], op=ALU.mult),
                     reads=[lru_f], writes=[t_rl])
                k.op("dve", lambda e: e.tensor_tensor(out=t_sq.ap, in0=t_sq.ap, in1=t_rl.ap, op=ALU.add),
                     reads=[t_sq, t_rl], writes=[t_sq])

        def lru_finish():
            k.op("dve", lambda e: e.tensor_copy(out=t_sqb.ap, in_=t_sq.ap), reads=[t_sq], writes=[t_sqb])
            ps_n = gbank()
            k.op("pe", lambda e: e.matmul(ps_n.ap, lhsT=ones_bf.ap, rhs=t_sqb.ap, start=True, stop=True),
                 reads=[ones_bf, t_sqb], writes=[ps_n])
            k.op("act", lambda e: e.activation(out=t_rl.ap, in_=ps_n.ap, func=AF.Ln, scale=1.0 / 512.0, bias=epst[:, 0:1]),
                 reads=[ps_n, epst], writes=[t_rl])
            k.op("act", lambda e: e.activation(out=t_rl.ap, in_=t_rl.ap, func=AF.Exp, scale=-0.5), reads=[t_rl], writes=[t_rl])
            for c in range(LRU_C):
                k.op("dve", lambda e, c=c: e.scalar_tensor_tensor(out=lru_nb[:, c, :], in0=lru_f[:, c, :], scalar=lvec[:, c, 3:4],
                                                                 in1=t_rl.ap, op0=ALU.mult, op1=ALU.mult),
                     reads=[lru_f, lvec, t_rl], writes=[lru_nb])

        def attention_head(b, i, h):
            nkb = 4 * i + 4
            pairs = [(kb, c) for kb in range(nkb) for c in range(2)]
            started = set()

            def qk2(m):
                kb = m
                r = kb - 4 * i
                qlo = max(0, r) * 128
                j = m % 2
                E = Et[j]
                for c in range(2):
                    psb = pbank[2 * j + c]
                    last = (c == 1)
                    k.op("pe", lambda e, c=c, psb=psb: e.matmul(psb[:, qlo:QB], lhsT=kT_blk[kb // 4][:, h, (kb % 4) * 128:(kb % 4 + 1) * 128],
                                                               rhs=qT[:, c, h, qlo:QB], start=True, stop=(r < 0)),
                         reads=[kT_blk[kb // 4], qT], writes=[psb], signal=(last and r < 0))
                    if r >= 0:
                        k.op("pe", lambda e, psb=psb: e.matmul(psb[:, qlo:qlo + 128], lhsT=ident.ap, rhs=negtri.ap, start=False, stop=True),
                             reads=[ident, negtri], writes=[psb], signal=last)
                sv = psall[:, 2 * j * 512:(2 * j + 2) * 512].rearrange("p (c q) -> p c q", c=2)
                k.op("act", lambda e: e.activation(out=E[:, :, qlo:QB], in_=sv[:, :, qlo:QB], func=AF.Exp),
                     reads=[pbank[2 * j], pbank[2 * j + 1]], writes=[E])

            def av2(m):
                kb = m
                r = kb - 4 * i
                s0 = max(0, r)
                E = Et[m % 2]
                for c in range(2):
                    for s in range(s0, 4):
                        bk = acc_bank(c, s)
                        first = bk.name not in started
                        started.add(bk.name)
                        k.op("pe", lambda e, c=c, s=s, first=first: e.matmul(acc_ap(c, s), lhsT=E[:, c, s * 128:(s + 1) * 128],
                                                                           rhs=V_blk[kb // 4][:, kb % 4, h, 0:129],
                                                                           start=first, stop=(kb == nkb - 1), skip_group_check=True),
                             reads=[E, V_blk[kb // 4]], writes=[bk], signal=(c == 1 and s == 3))

            qk2(0)
            for m in range(nkb):
                if m + 1 < nkb:
                    qk2(m + 1)
                av2(m)
            for (sl, ns, banks) in ((slice(0, 3), 3, (pbank[4], pbank[5])), (slice(3, 4), 1, (pbank[6], pbank[6]))):
                if ns == 3:
                    a0 = pbank[4].ap[:, 0:387].rearrange("p (s d) -> p s d", d=129)
                    a1 = pbank[5].ap[:, 0:387].rearrange("p (s d) -> p s d", d=129)
                else:
                    a0 = pbank[6].ap[:, 0:129].rearrange("p (s d) -> p s d", d=129)
                    a1 = pbank[6].ap[:, 129:258].rearrange("p (s d) -> p s d", d=129)
                rl = list(set(banks))
                ri0 = e_st[:, 0:ns].unsqueeze(2)
                ri1 = e_st[:, 3:3 + ns].unsqueeze(2)
                k.op("dve", lambda e: e.reciprocal(out=ri0, in_=a0[:, :, 128:129]), reads=rl, writes=[e_st])
                k.op("dve", lambda e: e.reciprocal(out=ri1, in_=a1[:, :, 128:129]), reads=rl, writes=[e_st])
                k.op("dve", lambda e: e.tensor_scalar(out=e_st[:, 3:3 + ns], in0=e_st[:, 3:3 + ns], scalar1=nlam_col[:, 0:1],
                                                      scalar2=None, op0=ALU.mult), reads=[e_st, nlam_col], writes=[e_st])
                k.op("dve", lambda e: e.tensor_tensor(out=e_o1[:, 0:ns, :], in0=a0[:, :, 0:128], in1=ri0.to_broadcast([128, ns, 128]),
                                                      op=ALU.mult), reads=rl + [e_st], writes=[e_o1])
                k.op("dve", lambda e: e.tensor_tensor(out=e_t2[:, 0:ns, :], in0=a1[:, :, 0:128], in1=ri1.to_broadcast([128, ns, 128]),
                                                      op=ALU.mult), reads=rl + [e_st], writes=[e_t2])
                k.op("dve", lambda e: e.tensor_tensor(out=e_o1[:, 0:ns, :], in0=e_o1[:, 0:ns, :], in1=e_t2[:, 0:ns, :], op=ALU.add),
                     reads=[e_o1, e_t2], writes=[e_o1])
                k.op("dve", lambda e: e.tensor_tensor(out=e_sq[:, 0:ns, :], in0=e_o1[:, 0:ns, :], in1=e_o1[:, 0:ns, :], op=ALU.mult),
                     reads=[e_o1], writes=[e_sq])
                k.op("dve", lambda e: e.tensor_reduce(out=e_st[:, 6:6 + ns], in_=e_sq[:, 0:ns, :], axis=AX.X, op=ALU.add),
                     reads=[e_sq], writes=[e_st])
                rstd_pow(e_st[:, 6:6 + ns], e_st[:, 6:6 + ns], [e_st], 1.0 / DV, ns)
                k.op("dve", lambda e: e.tensor_tensor(out=e_o1[:, 0:ns, :], in0=e_o1[:, 0:ns, :],
                                                      in1=e_st[:, 6:6 + ns].unsqueeze(2).to_broadcast([128, ns, 128]), op=ALU.mult),
                     reads=[e_o1, e_st], writes=[e_o1])
                k.op("dve", lambda e: e.tensor_tensor(out=attn_n[:, sl, h * 128:(h + 1) * 128], in0=e_o1[:, 0:ns, :],
                                                       in1=subg_bc.ap.unsqueeze(1).to_broadcast([128, ns, 128]), op=ALU.mult),
                     reads=[e_o1, subg_bc], writes=[attn_n])

        def phase_a(b, i):
            t0 = b * S + i * QB
            for s in range(4):
                xt = xb[s % 2]
                k.dma("sp", xt.ap, x_in.ap[t0 + s * 128:t0 + (s + 1) * 128, :], writes=[xt], sem="xb%d" % (s % 2))
                k.op("act", lambda e, xt=xt, s=s: e.activation(out=junk.ap, in_=xt.ap, func=AF.Square, accum_out=stat[:, s:s + 1]),
                     reads=[xt], writes=[junk, stat])
                rstd_pow(stat[:, s:s + 1], stat[:, s:s + 1], [stat], 1.0 / D, 1)
                k.op("act", lambda e, xt=xt, s=s: e.activation(out=xn[:, s, :], in_=xt.ap, func=AF.Copy, scale=stat[:, s:s + 1]),
                     reads=[xt, stat], writes=[xn])

        blocks = [(bb, ii) for bb in range(NB) for ii in range(NBLK)]
        phase_a(0, 0)
        for b in range(NB):
            k.op("dve", lambda e: e.memset(hstate.ap, 0.0), writes=[hstate])
            k.op("dve", lambda e: e.memset(xr_ext[:, :, 0:3], 0.0), writes=[xr_ext])
            k.dma("sp", g1_cur.ap, modrow_d.ap[2, b], reads=[modrow_d], writes=[g1_cur], sem="g1ld")
            for i in range(NBLK):
                t0 = b * S + i * QB
                gstate["banks"] = list(range(8))
                for kk in range(8):
                    pb = gbank()
                    pv = bank_bf(pb)[:, 0:512].rearrange("p (s t) -> p s t", t=128)
                    for s in range(4):
                        k.op("pe", lambda e, s=s, kk=kk, pv=pv: e.transpose(pv[:, s, :], xn[:, s, kk * 128:(kk + 1) * 128], ident.ap),
                             reads=[xn, ident], writes=[pb], signal=(s == 3))
                    k.op("dve", lambda e, kk=kk, pb=pb: e.tensor_scalar(out=hT[:, kk, :], in0=bank_bf(pb)[:, 0:512],
                                                                       scalar1=A1[:, b, kk:kk + 1], scalar2=B1[:, b, kk:kk + 1],
                                                                       op0=ALU.mult, op1=ALU.add),
                         reads=[pb, A1, B1], writes=[hT])
                def c_mm(s):
                    pbs = []
                    for g in range(3):
                        pb = gbank()
                        for kk in range(8):
                            k.op("pe", lambda e, kk=kk, s=s, g=g, pb=pb: e.matmul(pb.ap, lhsT=hT[:, kk, s * 128:(s + 1) * 128],
                                                                             rhs=w_in_sb[:, kk, g * 512:(g + 1) * 512],
                                                                             start=(kk == 0), stop=(kk == 7)),
                                 reads=[hT, w_in_sb], writes=[pb], signal=(kk == 7))
                        pbs.append(pb)
                    return pbs

                def c_norm(s, pbs):
                    par = s % 2
                    tmps = [(gbank(), st8_l2[par][g], n_qn_l2[par][g]) for g in range(2)]
                    for g in range(2):
                        nsq = tmps[g][0]
                        k.op("act", lambda e, pb=pbs[g], nsq=nsq: e.activation(out=nsq.ap, in_=pb.ap, func=AF.Square), reads=[pbs[g]], writes=[nsq])
                    k.op("act", lambda e, s=s, pb=pbs[2]: e.activation(out=V_blk[i][:, s, :, 0:128],
                                                                in_=pb.ap.rearrange("p (h d) -> p h d", d=128), func=AF.Copy),
                         reads=[pbs[2]], writes=[V_blk[i]])
                    for g in range(2):
                        nsq, st8, n_qn = tmps[g]
                        k.op("dve", lambda e, nsq=nsq, st8=st8: e.tensor_reduce(out=st8.ap, in_=nsq.ap.rearrange("p (g d) -> p g d", d=DK),
                                                              axis=AX.X, op=ALU.add), reads=[nsq], writes=[st8])
                    for g in range(2):
                        st8 = tmps[g][1]
                        k.op("act", lambda e, st8=st8: e.activation(out=st8.ap, in_=st8.ap, func=AF.Ln, scale=1.0 / DK, bias=epst[:, 0:1]),
                             reads=[st8, epst], writes=[st8])
                    for g in range(2):
                        st8 = tmps[g][1]
                        k.op("act", lambda e, st8=st8: e.activation(out=st8.ap, in_=st8.ap, func=AF.Exp, scale=-0.5), reads=[st8], writes=[st8])
                    for g in range(2):
                        nsq, st8, n_qn = tmps[g]
                        k.op("dve", lambda e, pb=pbs[g], st8=st8, n_qn=n_qn: e.tensor_tensor(out=n_qn.ap.rearrange("p (g d) -> p g d", d=DK),
                                                                  in0=pb.ap.rearrange("p (g d) -> p g d", d=DK),
                                                                  in1=st8.ap.unsqueeze(2).to_broadcast([128, 8, DK]), op=ALU.mult),
                             reads=[pbs[g], st8], writes=[n_qn])
                    return tmps

                def c_tr(s, tmps):
                    for g in range(2):
                        n_qn = tmps[g][2]
                        pt = gbank()
                        ptv = bank_bf(pt)[:, 0:512].rearrange("p (h t) -> p h t", t=128)
                        for hh in range(H):
                            k.op("pe", lambda e, hh=hh, ptv=ptv, n_qn=n_qn: e.transpose(ptv[:, hh, :], n_qn[:, hh * 128:(hh + 1) * 128], ident.ap),
                                 reads=[n_qn, ident], writes=[pt], signal=(hh == H - 1))
                        if g == 0:
                            for c in range(2):
                                k.op("dve", lambda e, ptv=ptv, s=s, c=c: e.tensor_scalar(
                                    out=qT[c * 64:(c + 1) * 64, c, :, s * 128:(s + 1) * 128], in0=ptv[c * 64:(c + 1) * 64],
                                    scalar1=qg[c * 64:(c + 1) * 64, 0:1], scalar2=None, op0=ALU.mult),
                                    reads=[pt, qg], writes=[qT])
                        else:
                            k.op("dve", lambda e, ptv=ptv, s=s: e.tensor_scalar(
                                out=kT_blk[i][:, :, s * 128:(s + 1) * 128], in0=ptv, scalar1=kg[:, 0:1], scalar2=None, op0=ALU.mult),
                                reads=[pt, kg], writes=[kT_blk[i]])

                pend = None
                for s in range(4):
                    pbs = c_mm(s)
                    if pend is not None:
                        c_tr(*pend)
                    pend = (s, c_norm(s, pbs))
                c_tr(*pend)
                if full:
                    sched = [0, 1, 1, 2, 2, 3, 3, 4]
                    for ee in range(sched[i]):
                        e_id = b * 16 + sum(sched[:i]) + ee
                        convert_expert(e_id, e_id == 0, after=[kT_blk[i]])
                gstate["banks"] = [7]
                lru_stage_a(b, i, 0)
                for h in range(H):
                    attention_head(b, i, h)
                    lru_stage_b(b, i, h)
                    if h + 1 < H:
                        lru_stage_a(b, i, h + 1)
                gstate["banks"] = list(range(8))
                for cc in range(4):
                    pb = gbank()
                    pv = bank_bf(pb)[:, 0:512].rearrange("p (s t) -> p s t", t=128)
                    for s in range(4):
                        k.op("pe", lambda e, s=s, cc=cc, pv=pv: e.transpose(pv[:, s, :], attn_n[:, s, cc * 128:(cc + 1) * 128], ident.ap),
                             reads=[attn_n, ident], writes=[pb], signal=(s == 3))
                    k.op("act", lambda e, cc=cc, pb=pb: e.activation(out=attnT[:, cc, :], in_=bank_bf(pb)[:, 0:512], func=AF.Copy),
                         reads=[pb], writes=[attnT])
                lru_finish()
                nxt = b * NBLK + i + 1
                if nxt < len(blocks):
                    phase_a(*blocks[nxt])
                for s in range(4):
                    xt = xb[s % 2]
                    k.dma("sp", xt.ap, x_in.ap[t0 + s * 128:t0 + (s + 1) * 128, :], writes=[xt], sem="xb%d" % (s % 2))
                    for half in range(2):
                        pb = gbank()
                        for cc in range(8):
                            lhs = attnT[:, cc, s * 128:(s + 1) * 128] if cc < 4 else lru_nb[:, cc - 4, s * 128:(s + 1) * 128]
                            k.op("pe", lambda e, cc=cc, lhs=lhs, half=half, pb=pb: e.matmul(
                                pb.ap, lhsT=lhs, rhs=w_out_sb[:, cc, half * 512:(half + 1) * 512], start=(cc == 0), stop=(cc == 7)),
                                reads=[attnT, lru_nb, w_out_sb], writes=[pb], signal=(cc == 7))
                        k.op("dve", lambda e, half=half, pb=pb: e.tensor_tensor(out=t_mix[:, half * 512:(half + 1) * 512], in0=pb.ap,
                                                                             in1=g1_cur[:, half * 512:(half + 1) * 512], op=ALU.mult),
                             reads=[pb, g1_cur], writes=[t_mix])
                    k.op("dve", lambda e, xt=xt: e.tensor_tensor(out=xt.ap, in0=xt.ap, in1=t_mix.ap, op=ALU.add),
                         reads=[xt, t_mix], writes=[xt])
                    k.dma("sp", x1_dst.ap[t0 + s * 128:t0 + (s + 1) * 128, :], xt.ap, reads=[xt], writes=[x1_dst], sem="x1st%d" % (s % 2))
        k_all_quiesce(k)

    if stop_after == "p1":
        k.finish()
        return nc

    es_w.close()

    h2_d = k.dram("h2_d", [T, D], BF16, track_waw=False)
    ys_d = k.dram("ys_d", [PSLOT, D], F32, track_waw=False)
    dest_i = k.sb("dest_i", [128, NTILE, TOPK], I32)
    gate_s = k.sb("gate_s", [128, NTILE, TOPK], F32)

    with ExitStack() as es2:
        def sb2(name, shape, dtype):
            return Tl(name, es2.enter_context(nc.sbuf_tensor(name, list(shape), dtype)).ap())

        A2r = [sb2("A2r%d" % b, [128, D], F32) for b in range(NB)]
        B2r = [sb2("B2r%d" % b, [128, D], F32) for b in range(NB)]
        n2row = sb2("n2row", [128, D], F32)
        x1t = [sb2("x1t%d" % i, [128, D], F32) for i in range(2)]
        junk2 = sb2("junk2", [128, D], BF16)
        h2f_l = [sb2("h2f%d" % i, [128, D], F32) for i in range(2)]
        h2T_l = [sb2("h2T%d" % i, [128, 8, 128], BF16) for i in range(2)]
        st2_l = [sb2("st2_%d" % i, [128, 8], F32) for i in range(2)]
        h2b = [sb2("h2b%d" % i, [128, D], BF16) for i in range(2)]
        rw_f = sb2("rw_f", [128, 8, NE], F32)
        rw_b = sb2("rw_b", [128, 8, NE], BF16)
        rb_bc = sb2("rb_bc", [128, NE], F32)
        U_bf = sb2("U_bf", [128, 128], BF16)
        logit = sb2("logit", [128, NTILE, NE], F32)
        mask = sb2("mask", [128, NTILE, NE], F32)
        mask_b = sb2("mask_b", [128, NTILE, NE], BF16)
        pref_b = sb2("pref_b", [128, NTILE + 1, NE], BF16)
        rank = sb2("rank", [128, NTILE, NE], F32)
        G = sb2("G", [128, NTILE, NE], F32)
        tmpA = sb2("tmpA", [128, NTILE, NE], F32)
        tmpB = sb2("tmpB", [128, NTILE, NE], F32)
        mx8 = sb2("mx8", [128, NTILE, 8], F32)
        cexp2 = sb2("cexp2", [128, 2], F32)
        ssum = sb2("ssum", [128, NTILE], F32)
        cnt = sb2("cnt", [128, NE], F32)
        cnt_i = sb2("cnt_i", [128, NE], I32)
        padf = sb2("padf", [128, NE], F32)
        pend = sb2("pend", [128, NE], F32)
        pstart = sb2("pstart", [128, NE], F32)
        ones32 = sb2("ones32", [128, NE], F32)
        jv = sb2("jv", [128, NMB], F32)
        cmpt = sb2("cmpt", [128, NMB, NE], F32)
        bexp_f = sb2("bexp_f", [128, NMB], F32)
        pidx = sb2("pidx", [128, 1], F32)
        same2 = sb2("same2", [128, NMB], F32)
        dest_f = sb2("dest_f", [128, NTILE, TOPK], F32)

        k.op("dve", lambda e: e.memset(cexp2[:, 0:1], -0.5), writes=[cexp2])
        k.dma("sp", n2row.ap, din("n2g_row").ap.to_broadcast([128, D]), writes=[n2row], sem="c0")
        k.dma("sp", rb_bc.ap, din("router_b").ap.to_broadcast([128, NE]), writes=[rb_bc], sem="c1")
        k.dma("sp", rw_f.ap, din("router_w").ap.rearrange("(k p) e -> p k e", p=128), writes=[rw_f], sem="c2")
        k.op("dve", lambda e: e.tensor_copy(out=rw_b.ap, in_=rw_f.ap), reads=[rw_f], writes=[rw_b])
        for b in range(NB):
            k.dma("sp", B2r[b].ap, modrow_d.ap[3, b], reads=[modrow_d], writes=[B2r[b]], sem="c3", join=(b > 0))
            k.dma("sp", A2r[b].ap, modrow_d.ap[4, b], reads=[modrow_d], writes=[A2r[b]], sem="c3", join=True)
        for b in range(NB):
            k.op("dve", lambda e, b=b: e.scalar_tensor_tensor(out=A2r[b].ap, in0=A2r[b].ap, scalar=1.0, in1=n2row.ap,
                                                              op0=ALU.add, op1=ALU.mult), reads=[A2r[b], n2row], writes=[A2r[b]])
        k.op("dve", lambda e: e.tensor_scalar(out=U_bf.ap, in0=iot.ap, scalar1=0.0, scalar2=None, op0=ALU.is_gt),
             reads=[iot], writes=[U_bf])
        k.op("pool", lambda e: e.memset(pref_b[:, 0, :], 0.0), writes=[pref_b])
        k.op("pool", lambda e: e.memset(ones32.ap, 1.0), writes=[ones32])

        p2b = {"n": 0}

        def gb2():
            b = pbank[p2b["n"] % 8]
            p2b["n"] += 1
            return b

        def p2_stage1(t):
            b = t // (NTILE // NB)
            xt = x1t[t % 2]
            hb = h2b[t % 2]
            h2f, h2T, st2 = h2f_l[t % 2], h2T_l[t % 2], st2_l[t % 2]
            k.dma("sp", xt.ap, x1_d.ap[t * 128:(t + 1) * 128, :], reads=[x1_d], writes=[xt], sem="x1t%d" % (t % 2))
            k.op("act", lambda e, xt=xt, st2=st2: e.activation(out=junk2.ap, in_=xt.ap, func=AF.Square, accum_out=st2[:, 0:1]),
                 reads=[xt], writes=[junk2, st2])
            rstd_act(st2[:, 0:1], st2[:, 0:1], [st2], 1.0 / D)
            k.op("dve", lambda e, xt=xt, b=b, h2f=h2f, st2=st2: e.scalar_tensor_tensor(out=h2f.ap, in0=xt.ap, scalar=st2[:, 0:1], in1=A2r[b].ap,
                                                                   op0=ALU.mult, op1=ALU.mult),
                 reads=[xt, st2, A2r[b]], writes=[h2f])
            k.op("dve", lambda e, hb=hb, b=b, h2f=h2f: e.tensor_tensor(out=hb.ap, in0=h2f.ap, in1=B2r[b].ap, op=ALU.add),
                 reads=[h2f, B2r[b]], writes=[hb])
            k.dma("pool", h2_d.ap[t * 128:(t + 1) * 128, :], hb.ap, reads=[hb], writes=[h2_d], sem="h2st%d" % (t % 2))

        def p2_stage1b(t):
            hb = h2b[t % 2]
            h2T = h2T_l[t % 2]
            pb = gb2()
            pv = bank_bf(pb).rearrange("p (k t) -> p k t", t=128)
            for kk in range(8):
                k.op("pe", lambda e, kk=kk, pv=pv, hb=hb: e.transpose(pv[:, kk, :], hb[:, kk * 128:(kk + 1) * 128], ident.ap),
                     reads=[hb, ident], writes=[pb], signal=(kk == 7))
            k.op("act", lambda e, pb=pb, h2T=h2T: e.activation(out=h2T.ap.rearrange("p k t -> p (k t)"), in_=bank_bf(pb), func=AF.Copy),
                 reads=[pb], writes=[h2T])

        def p2_stage2(t):
            h2T = h2T_l[t % 2]
            pl = gb2()
            for kk in range(8):
                k.op("pe", lambda e, kk=kk, pl=pl, h2T=h2T: e.matmul(pl[:, 0:NE], lhsT=h2T[:, kk, :], rhs=rw_b[:, kk, :],
                                                          start=(kk == 0), stop=(kk == 7)),
                     reads=[h2T, rw_b], writes=[pl], signal=(kk == 7))
            k.op("dve", lambda e, pl=pl, t=t: e.tensor_tensor(out=logit[:, t, :], in0=pl[:, 0:NE], in1=rb_bc.ap, op=ALU.add),
                 reads=[pl, rb_bc], writes=[logit])
            k.op("dve", lambda e, t=t: e.max(out=mx8[:, t, :], in_=logit[:, t, :]), reads=[logit], writes=[mx8])
            k.op("dve", lambda e, t=t: e.tensor_scalar(out=mask[:, t, :], in0=logit[:, t, :], scalar1=mx8[:, t, 3:4], scalar2=None,
                                                       op0=ALU.is_ge), reads=[logit, mx8], writes=[mask])
            k.op("dve", lambda e, t=t: e.tensor_copy(out=mask_b[:, t, :], in_=mask[:, t, :]), reads=[mask], writes=[mask_b])
            k.op("dve", lambda e, t=t: e.tensor_tensor(out=pref_b[:, t + 1, :], in0=pref_b[:, t, :], in1=mask_b[:, t, :], op=ALU.add),
                 reads=[pref_b, mask_b], writes=[pref_b])
            pr = gb2()
            k.op("pe", lambda e, pr=pr, t=t: e.matmul(pr[:, 0:NE], lhsT=U_bf.ap, rhs=mask_b[:, t, :], start=True, stop=False),
                 reads=[U_bf, mask_b], writes=[pr], signal=False)
            k.op("pe", lambda e, pr=pr, t=t: e.matmul(pr[:, 0:NE], lhsT=ones_bf.ap, rhs=pref_b[:, t, :], start=False, stop=True),
                 reads=[ones_bf, pref_b], writes=[pr])
            k.op("act", lambda e, pr=pr, t=t: e.activation(out=rank[:, t, :], in_=pr[:, 0:NE], func=AF.Copy), reads=[pr], writes=[rank])

        p2_stage1(0)
        p2_stage1b(0)
        for t in range(NTILE):
            if t + 1 < NTILE:
                p2_stage1(t + 1)
            p2_stage2(t)
            if t + 1 < NTILE:
                p2_stage1b(t + 1)
        pc = gb2()
        k.op("pe", lambda e: e.matmul(pc[:, 0:NE], lhsT=ones_bf.ap, rhs=pref_b[:, NTILE, :], start=True, stop=True),
             reads=[ones_bf, pref_b], writes=[pc])
        k.op("dve", lambda e: e.tensor_scalar(out=cnt.ap, in0=pc[:, 0:NE], scalar1=float(MB - 1), scalar2=None, op0=ALU.add),
             reads=[pc], writes=[cnt])
        k.op("dve", lambda e: e.tensor_copy(out=cnt_i.ap, in_=cnt.ap), reads=[cnt], writes=[cnt_i])
        k.op("dve", lambda e: e.tensor_scalar(out=cnt_i.ap, in0=cnt_i.ap, scalar1=int(math.log2(MB)), scalar2=None, op0=ALU.arith_shift_right),
             reads=[cnt_i], writes=[cnt_i])
        k.op("dve", lambda e: e.tensor_copy(out=padf.ap, in_=cnt_i.ap), reads=[cnt_i], writes=[padf])
        k.op("dve", lambda e: e.tensor_scalar(out=padf.ap, in0=padf.ap, scalar1=float(MB), scalar2=None, op0=ALU.mult),
             reads=[padf], writes=[padf])
        k.op("dve", lambda e: e.tensor_tensor_scan(out=pend.ap, data0=ones32.ap, data1=padf.ap, initial=0.0, op0=ALU.mult, op1=ALU.add),
             reads=[ones32, padf], writes=[pend])
        k.op("dve", lambda e: e.tensor_tensor(out=pstart.ap, in0=pend.ap, in1=padf.ap, op=ALU.subtract),
             reads=[pend, padf], writes=[pstart])
        k.op("pool", lambda e: e.iota(jv.ap, [[MB, NMB]], base=0, channel_multiplier=0, allow_small_or_imprecise_dtypes=True),
             writes=[jv])
        k.op("dve", lambda e: e.tensor_tensor(out=cmpt.ap, in0=pend.ap.unsqueeze(1).to_broadcast([128, NMB, NE]),
                                              in1=jv.ap.unsqueeze(2).to_broadcast([128, NMB, NE]), op=ALU.is_le),
             reads=[pend, jv], writes=[cmpt])
        k.op("dve", lambda e: e.tensor_reduce(out=bexp_f.ap, in_=cmpt.ap, axis=AX.X, op=ALU.add), reads=[cmpt], writes=[bexp_f])
        k.op("dve", lambda e: e.tensor_scalar(out=bexp_f.ap, in0=bexp_f.ap, scalar1=float(NE - 1), scalar2=None, op0=ALU.min),
             reads=[bexp_f], writes=[bexp_f])
        k.op("dve", lambda e: e.tensor_copy(out=idxb.ap, in_=bexp_f.ap), reads=[bexp_f], writes=[idxb])
        k.op("dve", lambda e: e.memset(same2.ap, 0.0), writes=[same2])
        k.op("dve", lambda e: e.tensor_tensor(out=same2[:, 2:NMB], in0=bexp_f[:, 2:NMB], in1=bexp_f[:, 0:NMB - 2], op=ALU.is_equal),
             reads=[bexp_f], writes=[same2])
        k.op("pool", lambda e: e.iota(pidx.ap, [[0, 1]], base=0, channel_multiplier=1, allow_small_or_imprecise_dtypes=True),
             writes=[pidx])
        k.op("dve", lambda e: e.tensor_scalar(out=bexp_f.ap, in0=bexp_f.ap, scalar1=128.0, scalar2=pidx[:, 0:1],
                                              op0=ALU.mult, op1=ALU.add), reads=[bexp_f, pidx], writes=[bexp_f])
        k.op("dve", lambda e: e.tensor_copy(out=idxw.ap, in_=bexp_f.ap), reads=[bexp_f], writes=[idxw])
        k.op("dve", lambda e: e.scalar_tensor_tensor(out=bexp_f.ap, in0=same2.ap, scalar=1.0e6, in1=bexp_f.ap, op0=ALU.mult, op1=ALU.add),
             reads=[same2, bexp_f], writes=[bexp_f])
        k.op("dve", lambda e: e.tensor_copy(out=idxs.ap, in_=bexp_f.ap), reads=[bexp_f], writes=[idxs])
        k.op("dve", lambda e: e.tensor_tensor(out=tmpA.ap, in0=logit.ap, in1=mx8[:, :, 0:1].to_broadcast([128, NTILE, NE]),
                                              op=ALU.subtract), reads=[logit, mx8], writes=[tmpA])
        k.op("act", lambda e: e.activation(out=tmpA.ap, in_=tmpA.ap, func=AF.Exp), reads=[tmpA], writes=[tmpA])
        k.op("dve", lambda e: e.tensor_tensor(out=tmpA.ap, in0=tmpA.ap, in1=mask.ap, op=ALU.mult), reads=[tmpA, mask], writes=[tmpA])
        k.op("dve", lambda e: e.tensor_reduce(out=ssum.ap, in_=tmpA.ap, axis=AX.X, op=ALU.add), reads=[tmpA], writes=[ssum])
        k.op("dve", lambda e: e.reciprocal(out=ssum.ap, in_=ssum.ap), reads=[ssum], writes=[ssum])
        k.op("dve", lambda e: e.tensor_tensor(out=G.ap, in0=tmpA.ap, in1=ssum.ap.unsqueeze(2).to_broadcast([128, NTILE, NE]),
                                              op=ALU.mult), reads=[tmpA, ssum], writes=[G])
        k.op("dve", lambda e: e.tensor_tensor(out=rank.ap, in0=rank.ap, in1=pstart.ap.unsqueeze(1).to_broadcast([128, NTILE, NE]),
                                              op=ALU.add), reads=[rank, pstart], writes=[rank])
        for kq in range(TOPK):
            k.op("dve", lambda e, kq=kq: e.tensor_tensor(out=tmpB.ap, in0=logit.ap,
                                                         in1=mx8[:, :, kq:kq + 1].to_broadcast([128, NTILE, NE]), op=ALU.is_equal),
                 reads=[logit, mx8], writes=[tmpB])
            k.op("dve", lambda e: e.tensor_tensor(out=tmpA.ap, in0=tmpB.ap, in1=rank.ap, op=ALU.mult),
                 reads=[tmpB, rank], writes=[tmpA])
            k.op("dve", lambda e, kq=kq: e.tensor_reduce(out=dest_f[:, :, kq], in_=tmpA.ap, axis=AX.X, op=ALU.add),
                 reads=[tmpA], writes=[dest_f])
            k.op("dve", lambda e: e.tensor_tensor(out=tmpA.ap, in0=tmpB.ap, in1=G.ap, op=ALU.mult),
                 reads=[tmpB, G], writes=[tmpA])
            k.op("dve", lambda e, kq=kq: e.tensor_reduce(out=gate_s[:, :, kq], in_=tmpA.ap, axis=AX.X, op=ALU.add),
                 reads=[tmpA], writes=[gate_s])
        k.op("dve", lambda e: e.tensor_copy(out=dest_i.ap, in_=dest_f.ap), reads=[dest_f], writes=[dest_i])
        hsc = h2b + [sb2("h2sc%d" % i, [128, D], BF16) for i in range(2)]
        for t in range(NTILE):
            hb = hsc[t % 4]
            k.dma("sp", hb.ap, h2_d.ap[t * 128:(t + 1) * 128, :], reads=[h2_d], writes=[hb], sem="h2ld%d" % (t % 4))
            for kq in range(TOPK):
                k.dma("pool", xs_d.ap, hb.ap, reads=[hb, dest_i], writes=[xs_d], sem="scat%d" % (t % 4), join=(kq > 0),
                      indirect=dict(out_offset=bass.IndirectOffsetOnAxis(ap=dest_i[:, t, kq:kq + 1], axis=0), in_offset=None))
        if stop_after == "p2":
            dbg = sb2("dbg2", [128, D], F32)
            k.op("dve", lambda e: e.memset(dbg.ap, 0.0), writes=[dbg])
            k.op("dve", lambda e: e.tensor_copy(out=dbg[:, 0:256], in_=dest_f.ap.rearrange("p t k -> p (t k)")), reads=[dest_f], writes=[dbg])
            k.op("dve", lambda e: e.tensor_copy(out=dbg[:, 256:512], in_=gate_s.ap.rearrange("p t k -> p (t k)")), reads=[gate_s], writes=[dbg])
            k.op("dve", lambda e: e.tensor_copy(out=dbg[:, 512:544], in_=pstart.ap), reads=[pstart], writes=[dbg])
            k.op("dve", lambda e: e.tensor_copy(out=dbg[:, 544:576], in_=pc[:, 0:NE]), reads=[pc], writes=[dbg])
            k.op("dve", lambda e: e.tensor_copy(out=dbg[:, 576:672], in_=bexp_f.ap), reads=[bexp_f], writes=[dbg])
            k.dma("sp", out_t.ap[0:128, :], dbg.ap, reads=[dbg], sem="ost")
            k.dma("sp", out_t.ap[128:256, :], logit.ap.rearrange("p t e -> p (t e)")[:, 0:1024], reads=[logit], sem="ost")
            k.dma("sp", out_t.ap[384:512, :], A2r[0].ap, reads=[A2r[0]], sem="ost")
            k.dma("sp", out_t.ap[512:640, :], B2r[0].ap, reads=[B2r[0]], sem="ost")
            dbg3 = sb2("dbg3", [128, D], F32)
            k.dma("sp", h2b[0].ap, h2_d.ap[0:128, :], reads=[h2_d], writes=[h2b[0]], sem="dbgl")
            k.op("dve", lambda e: e.tensor_copy(out=dbg3.ap, in_=h2b[0].ap), reads=[h2b[0]], writes=[dbg3])
            k.dma("sp", out_t.ap[256:384, :], dbg3.ap, reads=[dbg3], sem="ost")
        k_all_quiesce(k)

    if stop_after == "p2":
        k.finish()
        return nc

    with ExitStack() as es3:
        def sb3(name, shape, dtype):
            return Tl(name, es3.enter_context(nc.sbuf_tensor(name, list(shape), dtype)).ap())

        wgu = [sb3("wgu%d" % i, [128, 8, 2 * DFF], BF16) for i in range(2)]
        wdn = [sb3("wdn%d" % i, [128, 8, D], BF16) for i in range(2)]
        bgu = [sb3("bgu%d" % i, [128, 16], F32) for i in range(2)]
        bdn = [sb3("bdn%d" % i, [128, D], F32) for i in range(2)]
        xsb = [sb3("xsb%d" % i, [128, MB // 128, D], BF16) for i in range(2)]
        xgT_l = [sb3("xgT%d" % i, [128, 8, MB], BF16) for i in range(2)]
        xgT_r = [[Res("xgT%d_%d" % (i, kk)) for kk in range(8)] for i in range(2)]
        actT = sb3("actT", [128, 8, MB], BF16)
        actT_r = [Res("actT%d" % i) for i in range(8)]
        g_sb = [sb3("g_sb%d" % i, [128, MB], F32) for i in range(2)]
        sg_sb = [sb3("sg_sb%d" % i, [128, MB], F32) for i in range(2)]
        u_sb = [sb3("u_sb%d" % i, [128, MB], F32) for i in range(2)]
        ysb = [sb3("ysb%d" % i, [128, D], F32) for i in range(2)]

        bgu_v = din("b_gu_col").ap.rearrange("e p c -> (e p) c")
        reg_bc = nc.gpsimd.alloc_register("reg_bc")
        nc.gpsimd.reg_mov(reg_bc, NE * 128 - 1)
        bdn_v = din("b_dn").ap.rearrange("e o d -> (e o) d")
        p3b = {"n": 0}

        def gb3():
            b = pbank[p3b["n"] % 8]
            p3b["n"] += 1
            return b

        def p3_loads(j):
            wg, wd, bg, bd, xs = wgu[j % 2], wdn[j % 2], bgu[j % 2], bdn[j % 2], xsb[j % 2]
            iw = bass.IndirectOffsetOnAxis(ap=idxw[:, j:j + 1], axis=0)
            ib = bass.IndirectOffsetOnAxis(ap=idxb[:, j:j + 1], axis=0)
            isk = bass.IndirectOffsetOnAxis(ap=idxs[:, j:j + 1], axis=0)
            k.dma("pool", wg.ap.rearrange("p k f -> p (k f)"), wgu_bf_d.ap, reads=[wgu_bf_d, idxs], writes=[wg], sem="wgu%d" % (j % 2),
                  indirect=dict(out_offset=None, in_offset=isk, bounds_check=reg_bc, oob_is_err=False))
            k.dma("pool", wd.ap.rearrange("p k f -> p (k f)"), wdn_bf_d.ap, reads=[wdn_bf_d, idxs], writes=[wd], sem="wdn%d" % (j % 2),
                  indirect=dict(out_offset=None, in_offset=isk, bounds_check=reg_bc, oob_is_err=False))
            k.dma("pool", bg.ap, bgu_v, reads=[idxw], writes=[bg], sem="bgu%d" % (j % 2), indirect=dict(out_offset=None, in_offset=iw))
            k.dma("pool", bd.ap, bdn_v, reads=[idxb], writes=[bd], sem="bdn%d" % (j % 2), indirect=dict(out_offset=None, in_offset=ib))

        def p3_load_x(j):
            xs = xsb[j % 2]
            k.dma("sp", xs.ap, xs_d.ap[j * MB:(j + 1) * MB, :].rearrange("(s p) d -> p s d", p=128), reads=[xs_d], writes=[xs],
                  sem="xsb%d" % (j % 2))

        def p3_transposes(j):
            xs = xsb[j % 2]
            xgT = xgT_l[j % 2]
            xr_ = xgT_r[j % 2]
            for kk in range(8):
                pb = gb3()
                pv = bank_bf(pb)[:, 0:MB].rearrange("p (s t) -> p s t", t=128)
                for s in range(MB // 128):
                    k.op("pe", lambda e, s=s, kk=kk, pv=pv, xs=xs: e.transpose(pv[:, s, :], xs[:, s, kk * 128:(kk + 1) * 128], ident.ap),
                         reads=[xs, ident], writes=[pb], signal=(s == MB // 128 - 1))
                eng = "act" if kk % 2 == 0 else "dve"
                if eng == "act":
                    k.op("act", lambda e, kk=kk, pb=pb, xgT=xgT: e.activation(out=xgT[:, kk, :], in_=bank_bf(pb)[:, 0:MB], func=AF.Copy),
                         reads=[pb], writes=[xr_[kk]])
                else:
                    k.op("dve", lambda e, kk=kk, pb=pb, xgT=xgT: e.tensor_copy(out=xgT[:, kk, :], in_=bank_bf(pb)[:, 0:MB]),
                         reads=[pb], writes=[xr_[kk]])

        p3_loads(0)
        p3_load_x(0)
        p3_load_x(1)
        p3_transposes(0)
        for j in range(NMB):
            wg, wd, bg, bd = wgu[j % 2], wdn[j % 2], bgu[j % 2], bdn[j % 2]
            xgT = xgT_l[j % 2]
            xr_ = xgT_r[j % 2]
            if j + 1 < NMB:
                p3_loads(j + 1)
            for fc in range(8):
                pg = gb3()
                for kk in range(8):
                    k.op("pe", lambda e, kk=kk, fc=fc, pg=pg, wg=wg, xgT=xgT: e.matmul(pg[:, 0:MB], lhsT=wg[:, kk, fc * 128:(fc + 1) * 128], rhs=xgT[:, kk, :],
                                                                          start=(kk == 0), stop=(kk == 7)),
                         reads=[wg, xr_[kk]], writes=[pg], signal=(kk == 7))
                pu = gb3()
                for kk in range(8):
                    k.op("pe", lambda e, kk=kk, fc=fc, pu=pu, wg=wg, xgT=xgT: e.matmul(pu[:, 0:MB], lhsT=wg[:, kk, DFF + fc * 128:DFF + (fc + 1) * 128],
                                                                          rhs=xgT[:, kk, :], start=(kk == 0), stop=(kk == 7)),
                         reads=[wg, xr_[kk]], writes=[pu], signal=(kk == 7))
                gs, sgs, us = g_sb[fc % 2], sg_sb[fc % 2], u_sb[fc % 2]
                k.op("dve", lambda e, pg=pg, gs=gs, bg=bg, fc=fc: e.tensor_scalar(out=gs.ap, in0=pg[:, 0:MB], scalar1=bg[:, fc:fc + 1], scalar2=7.0,
                                                                               op0=ALU.add, op1=ALU.min), reads=[pg, bg], writes=[gs])
                k.op("act", lambda e, gs=gs, sgs=sgs: e.activation(out=sgs.ap, in_=gs.ap, func=AF.Sigmoid, scale=1.702),
                     reads=[gs], writes=[sgs])
                k.op("dve", lambda e, pu=pu, us=us, bg=bg, fc=fc: e.tensor_scalar(out=us.ap, in0=pu[:, 0:MB], scalar1=bg[:, 8 + fc:9 + fc], scalar2=7.0,
                                                                               op0=ALU.add, op1=ALU.min), reads=[pu, bg], writes=[us])
                k.op("dve", lambda e, us=us: e.tensor_scalar(out=us.ap, in0=us.ap, scalar1=-7.0, scalar2=1.0, op0=ALU.max, op1=ALU.add),
                     reads=[us], writes=[us])
                k.op("dve", lambda e, gs=gs, sgs=sgs: e.tensor_tensor(out=gs.ap, in0=gs.ap, in1=sgs.ap, op=ALU.mult),
                     reads=[gs, sgs], writes=[gs])
                k.op("dve", lambda e, gs=gs, us=us, fc=fc: e.tensor_tensor(out=actT[:, fc, :], in0=gs.ap, in1=us.ap, op=ALU.mult),
                     reads=[gs, us], writes=[actT_r[fc]])
            if j + 2 < NMB:
                p3_load_x(j + 2)
            if j + 1 < NMB:
                p3_transposes(j + 1)
            for s in range(MB // 128):
                yt = ysb[s % 2]
                for half in range(2):
                    pd = gb3()
                    for fc in range(8):
                        k.op("pe", lambda e, fc=fc, s=s, half=half, pd=pd, wd=wd: e.matmul(
                            pd.ap, lhsT=actT[:, fc, s * 128:(s + 1) * 128], rhs=wd[:, fc, half * 512:(half + 1) * 512],
                            start=(fc == 0), stop=(fc == 7)), reads=[actT_r[fc], wd], writes=[pd], signal=(fc == 7))
                    k.op("dve", lambda e, half=half, pd=pd, yt=yt, bd=bd: e.tensor_tensor(
                        out=yt[:, half * 512:(half + 1) * 512], in0=pd.ap, in1=bd[:, half * 512:(half + 1) * 512], op=ALU.add),
                        reads=[pd, bd], writes=[yt])
                k.dma("sp", ys_d.ap[j * MB + s * 128:j * MB + (s + 1) * 128, :], yt.ap, reads=[yt], writes=[ys_d], sem="yst%d" % (s % 2))
        k_all_quiesce(k)

    with ExitStack() as es4:
        def sb4(name, shape, dtype):
            return Tl(name, es4.enter_context(nc.sbuf_tensor(name, list(shape), dtype)).ap())

        g2bc = [sb4("g2bc%d" % b, [128, D], F32) for b in range(NB)]
        x1c = [sb4("x1c%d" % i, [128, D], F32) for i in range(2)]
        yk = [[sb4("yk%d_%d" % (i, q), [128, D], F32) for q in range(TOPK)] for i in range(2)]
        acc = [sb4("acc%d" % i, [128, D], F32) for i in range(2)]
        for b in range(NB):
            k.dma("sp", g2bc[b].ap, modrow_d.ap[5, b], reads=[modrow_d], writes=[g2bc[b]], sem="g2ld")
        def p4_loads(t):
            xt = x1c[t % 2]
            k.dma("sp", xt.ap, x1_d.ap[t * 128:(t + 1) * 128, :], reads=[x1_d], writes=[xt], sem="x1c%d" % (t % 2))
            for q in range(TOPK):
                y = yk[t % 2][q]
                k.dma("pool", y.ap, ys_d.ap, reads=[ys_d, dest_i], writes=[y], sem="yk%d_%d" % (t % 2, q),
                      indirect=dict(out_offset=None, in_offset=bass.IndirectOffsetOnAxis(ap=dest_i[:, t, q:q + 1], axis=0)))

        p4_loads(0)
        for t in range(NTILE):
            b = t // (NTILE // NB)
            xt = x1c[t % 2]
            ac = acc[t % 2]
            if t + 1 < NTILE:
                p4_loads(t + 1)
            k.op("dve", lambda e, t=t, ac=ac: e.tensor_scalar(out=ac.ap, in0=yk[t % 2][0].ap, scalar1=gate_s[:, t, 0:1], scalar2=None,
                                                             op0=ALU.mult), reads=[yk[t % 2][0], gate_s], writes=[ac])
            for q in range(1, TOPK):
                k.op("dve", lambda e, t=t, q=q, ac=ac: e.scalar_tensor_tensor(out=ac.ap, in0=yk[t % 2][q].ap, scalar=gate_s[:, t, q:q + 1],
                                                                             in1=ac.ap, op0=ALU.mult, op1=ALU.add),
                     reads=[yk[t % 2][q], gate_s, ac], writes=[ac])
            k.op("dve", lambda e, ac=ac, b=b: e.tensor_tensor(out=ac.ap, in0=ac.ap, in1=g2bc[b].ap, op=ALU.mult),
                 reads=[ac, g2bc[b]], writes=[ac])
            k.op("dve", lambda e, ac=ac, xt=xt: e.tensor_tensor(out=ac.ap, in0=ac.ap, in1=xt.ap, op=ALU.add),
                 reads=[ac, xt], writes=[ac])
            k.dma("sp", out_t.ap[t * 128:(t + 1) * 128, :], ac.ap, reads=[ac], sem="ost%d" % (t % 2))
        k_all_quiesce(k)

    k.finish()
    return nc


def k_all_quiesce(k):
    targets = []
    for en in ("pe", "act", "dve", "pool"):
        E = k.E[en]
        if E["ptok"] is not None:
            raise RuntimeError("pending unsignalled op at quiesce on " + en)
        if E["seq"] > 0:
            targets.append(Tok(E["sem"], E["seq"], en, "S_" + en))
    for name, (sem, cnt) in k.dsem.items():
        targets.append(Tok(sem, cnt, None, name))
    for en in ("pe", "act", "dve", "pool", "sp"):
        E = k.E[en]
        for t in targets:
            if t.eng == en:
                continue
            val = t.val
            if val > E["known"].get(t.key, 0):
                E["eng"].wait_ge(t.sem, val)
                E["known"][t.key] = val


def make_in_maps(inp):
    f = lambda a: np.ascontiguousarray(a, dtype=np.float32)
    x = inp["x"]
    maps = []
    col8 = lambda v: f(v.reshape(8, 128).T)
    lru_blk = np.zeros((128, 2, LRU_C, 128), np.float32)
    for gi, nm in enumerate(("lru_wa", "lru_wx")):
        w = inp[nm][0]
        for c in range(LRU_C):
            for hb in range(2):
                lru_blk[hb * 64:(hb + 1) * 64, gi, c, hb * 64:(hb + 1) * 64] = w[2 * c + hb]
    conv_col = np.zeros((128, LRU_C, 5), np.float32)
    for j in range(4):
        conv_col[:, :, j] = inp["conv_w"][0, j].reshape(LRU_C, 128).T
    conv_col[:, :, 4] = inp["conv_b"][0].reshape(LRU_C, 128).T
    lru_vec = np.zeros((128, LRU_C, 4), np.float32)
    for j, nm in enumerate(("lru_ba", "lru_bx", "lru_lambda", "lru_out_g")):
        lru_vec[:, :, j] = inp[nm][0].reshape(LRU_C, 128).T
    shared = {
        "ada_w": f(inp["ada_w"][0]),
        "ada_bT": f(inp["ada_b"][0].reshape(48, 128).T),
        "ada_b": f(inp["ada_b"][0].reshape(1, -1)),
        "n1g_col": col8(inp["norm1_g"][0]),
        "n2g_row": f(inp["norm2_g"][0].reshape(1, -1)),
        "w_in": f(inp["w_in"][0]),
        "qg_col": f(np.tile(inp["q_norm_g"][0], 2).reshape(128, 1)),
        "kg_col": f(np.tile(inp["k_norm_g"][0], 2).reshape(128, 1)),
        "lamv": f(np.concatenate([inp["lambda_q1"][0], inp["lambda_k1"][0], inp["lambda_q2"][0], inp["lambda_k2"][0]]).reshape(1, -1)),
        "subg_row": f(inp["attn_subln_g"][0].reshape(1, -1)),
        "conv_col": conv_col,
        "lru_blk": lru_blk,
        "lru_vec": lru_vec,
        "w_out": f(inp["w_out"][0]),
        "router_w": f(inp["router_w"][0]),
        "router_b": f(inp["router_b"][0].reshape(1, -1)),
        "w_gu": f(inp["w_gate_up"][0]),
        "b_gu_col": f(inp["b_gate_up"][0].reshape(NE, 16, 128).transpose(0, 2, 1)),
        "w_dn": f(inp["w_down"][0]),
        "b_dn": f(inp["b_down"][0].reshape(NE, 1, D)),
    }
    for c in range(NCORES):
        m = dict(shared)
        m["x"] = f(x[NB * c:NB * (c + 1)].reshape(T, D))
        cc = inp["c"][NB * c:NB * (c + 1)]
        m["cT"] = f(cc.reshape(NB, 8, 128).transpose(2, 1, 0))
        maps.append(m)
    return maps


_PROG_CACHE = {}


def kernel(**inputs):
    stop_after = inputs.pop("_stop_after", None)
    if stop_after not in _PROG_CACHE:
        _PROG_CACHE[stop_after] = build_program(stop_after)
    nc = _PROG_CACHE[stop_after]
    in_maps = [{kk: v for kk, v in m.items() if kk in nc._used_inputs} for m in make_in_maps(inputs)]
    res = run_bass_kernel_spmd(nc, in_maps, core_ids=list(range(NCORES)))
    outs = [np.asarray(r["out"]).reshape(NB, S, D) for r in res.results]
    return np.concatenate(outs, axis=0).astype(np.float32)
```

```python
import math
from contextlib import ExitStack

import numpy as np
import concourse.bass as bass
import concourse.mybir as mybir
from concourse.bass_utils import run_bass_kernel_spmd

F32 = mybir.dt.float32
BF16 = mybir.dt.bfloat16
I32 = mybir.dt.int32
U32 = mybir.dt.uint32
AF = mybir.ActivationFunctionType
ALU = mybir.AluOpType
AX = mybir.AxisListType

NCORES = 8
D = 1024
S = 4096
NB = 2
T = NB * S
NTILE = T // 128
QB = 512
NBLK = S // QB
H = 4
DK = 64
DV = 128
LRU_C = 4
NE = 32
TOPK = 4
DFF = 1024
MB = 256
NSLOT = T * TOPK
NMB = NSLOT // MB + NE
PSLOT = NMB * MB
EPS = 1e-6
LAM_INIT = 0.8 - 0.6 * math.exp(-0.3 * 0)
SAME_ENGINE_SYNC = True


class Tok:
    __slots__ = ("sem", "val", "eng", "key")

    def __init__(self, sem, val, eng, key):
        self.sem, self.val, self.eng, self.key = sem, val, eng, key


class Res:
    def __init__(self, name, track_waw=True):
        self.name = name
        self.w = None
        self.r = {}
        self.track_waw = track_waw


class Tl(Res):
    def __init__(self, name, ap, track_waw=True):
        super().__init__(name, track_waw)
        self.ap = ap

    def __getitem__(self, idx):
        return self.ap[idx]


class Alias(Tl):
    def __init__(self, parent, ap):
        self.parent = parent
        self.name = parent.name
        self.ap = ap
        self.track_waw = parent.track_waw

    @property
    def w(self):
        return self.parent.w

    @w.setter
    def w(self, v):
        self.parent.w = v

    @property
    def r(self):
        return self.parent.r

    @r.setter
    def r(self, v):
        self.parent.r = v


def alias(parent, shape, dtype, off_bytes=0):
    pdt_size = {F32: 4, BF16: 2, I32: 4, U32: 4}
    flat = parent.ap
    if len(flat.shape) > 2:
        names = " ".join("d%d" % i for i in range(len(flat.shape) - 1))
        flat = flat.rearrange("p %s -> p (%s)" % (names, names))
    psz = pdt_size[parent.ap.dtype]
    n = int(np.prod(shape))
    nbytes = n * pdt_size[dtype]
    a = flat[:, off_bytes // psz:(off_bytes + nbytes) // psz]
    if dtype != parent.ap.dtype:
        a = a.bitcast(dtype)
    if len(shape) > 1:
        names = " ".join("d%d" % i for i in range(len(shape)))
        kw = {"d%d" % i: int(shape[i]) for i in range(1, len(shape))}
        a = a.rearrange("p (%s) -> p %s" % (names, names), **kw)
    return Alias(parent, a)


class KB:
    def __init__(self, nc):
        self.nc = nc
        self.E = {}
        for name, eng in (("pe", nc.tensor), ("act", nc.scalar), ("dve", nc.vector),
                          ("pool", nc.gpsimd), ("sp", nc.sync)):
            self.E[name] = dict(eng=eng, sem=nc.alloc_semaphore("S_" + name), seq=0,
                                known={}, ptok=None)
        self.dsem = {}
        self.uid = 0

    def sb(self, name, shape, dtype, **kw):
        return Tl(name, self.nc.alloc_sbuf_tensor(name, list(shape), dtype).ap(), **kw)

    def ps(self, name, shape, dtype=F32):
        return Tl(name, self.nc.alloc_psum_tensor(name, list(shape), dtype).ap())

    def dram(self, name, shape, dtype, kind="Internal", **kw):
        return Tl(name, self.nc.dram_tensor(name, list(shape), dtype, kind=kind).ap(), **kw)

    def _deps(self, reads, writes):
        toks = []
        for r in reads:
            toks.append(r.w)
        for w in writes:
            if w.track_waw:
                toks.append(w.w)
                toks.extend(w.r.values())
        return toks

    def _wait_for(self, en, toks):
        E = self.E[en]
        need = {}
        for t in toks:
            if t is None:
                continue
            if t.eng == en and (en == "pe" or not SAME_ENGINE_SYNC):
                continue
            val = t.val
            if t.eng is None:
                val = self.dsem[t.key][1]
            elif val is None:
                if t.eng == en:
                    continue
                raise RuntimeError("dependency on unsignalled op of %s" % t.eng)
            if val > E["known"].get(t.key, 0) and val > need.get(t.key, (None, 0))[1]:
                need[t.key] = (t.sem, val)
        for key, (sem, val) in need.items():
            E["eng"].wait_ge(sem, val)
            E["known"][key] = val

    @staticmethod
    def _commit(tok, reads, writes):
        for r in reads:
            r.r[tok.key] = tok
        for w in writes:
            w.w = tok
            w.r = {}

    def op(self, en, fn, reads=(), writes=(), signal=True):
        E = self.E[en]
        self._wait_for(en, self._deps(reads, writes))
        inst = fn(E["eng"])
        if E["ptok"] is None:
            E["ptok"] = Tok(E["sem"], None, en, "S_" + en)
        tok = E["ptok"]
        self._commit(tok, reads, writes)
        if signal:
            E["seq"] += 1
            inst.then_inc(E["sem"], 1)
            tok.val = E["seq"]
            E["ptok"] = None
        return inst

    def dma(self, q, out, in_, reads=(), writes=(), sem=None, indirect=None, join=False, **kw):
        E = self.E[q]
        self._wait_for(q, self._deps(reads, writes))
        if sem not in self.dsem:
            self.dsem[sem] = [self.nc.alloc_semaphore("D_" + sem), 0]
        s = self.dsem[sem]
        if not join and s[1] > E["known"].get(sem, 0):
            E["eng"].wait_ge(s[0], s[1])
            E["known"][sem] = s[1]
        s[1] += 16
        if indirect is None:
            inst = E["eng"].dma_start(out=out, in_=in_, **kw)
        else:
            inst = E["eng"].indirect_dma_start(out=out, in_=in_, **indirect, **kw)
        inst.then_inc(s[0], 16)
        tok = Tok(s[0], s[1], None, sem)
        self._commit(tok, reads, writes)
        return inst

    def finish(self):
        sp = self.E["sp"]["eng"]
        for name, (sem, cnt) in self.dsem.items():
            if cnt > self.E["sp"]["known"].get(name, 0):
                sp.wait_ge(sem, cnt)
        for en in ("pe", "act", "dve", "pool"):
            E = self.E[en]
            if E["seq"] > 0:
                sp.wait_ge(E["sem"], E["seq"])


def build_program(stop_after=None):
    nc = bass.Bass("TRN2", target_bir_lowering=False)
    k = KB(nc)

    INSHAPES = {
        "x": [T, D], "cT": [128, 8, NB], "ada_w": [D, 6 * D], "ada_bT": [128, 48], "ada_b": [1, 6 * D],
        "n1g_col": [128, 8], "n2g_row": [1, D], "w_in": [D, 2560], "qg_col": [128, 1], "kg_col": [128, 1],
        "lamv": [1, 4 * DK], "subg_row": [1, DV], "conv_col": [128, LRU_C, 5], "lru_blk": [128, 2, LRU_C, 128],
        "lru_vec": [128, LRU_C, 4], "w_out": [D, D], "router_w": [D, NE], "router_b": [1, NE],
        "w_gu": [NE, D, 2 * DFF], "b_gu_col": [NE, 128, 16], "w_dn": [NE, DFF, D], "b_dn": [NE, 1, D],
    }
    used_inputs = {}

    def din(name):
        if name not in used_inputs:
            used_inputs[name] = Tl(name, nc.dram_tensor(name, list(INSHAPES[name]), F32, kind="ExternalInput").ap())
        return used_inputs[name]

    nc._used_inputs = used_inputs
    x_in = din("x")
    out_t = Tl("out", nc.dram_tensor("out", [T, D], F32, kind="ExternalOutput").ap(), track_waw=False)

    x1_d = k.dram("x1_d", [T, D], F32, track_waw=False)

    ident = k.sb("ident", [128, 128], BF16)
    tri = k.sb("tri", [128, 128], BF16)
    ones_bf = k.sb("ones_bf", [128, 128], BF16)
    iot = k.sb("iot", [128, 128], F32)
    k.op("pool", lambda e: e.iota(iot.ap, [[1, 128]], base=0, channel_multiplier=-1,
                                  allow_small_or_imprecise_dtypes=True), writes=[iot])
    k.op("dve", lambda e: e.tensor_scalar(out=ident.ap, in0=iot.ap, scalar1=0.0, scalar2=None,
                                          op0=ALU.is_equal), reads=[iot], writes=[ident])
    k.op("dve", lambda e: e.tensor_scalar(out=tri.ap, in0=iot.ap, scalar1=0.0, scalar2=None,
                                          op0=ALU.is_ge), reads=[iot], writes=[tri])
    k.op("dve", lambda e: e.memset(ones_bf.ap, 1.0), writes=[ones_bf])
    negtri = k.sb("negtri", [128, 128], BF16)
    k.op("dve", lambda e: e.tensor_scalar(out=negtri.ap, in0=iot.ap, scalar1=0.0, scalar2=-30000.0,
                                          op0=ALU.is_lt, op1=ALU.mult), reads=[iot], writes=[negtri])
    epst = k.sb("epst", [128, 2], F32)
    k.op("dve", lambda e: e.memset(epst[:, 0:1], float(EPS)), writes=[epst])
    k.op("dve", lambda e: e.memset(epst[:, 1:2], 1.0), writes=[epst])

    def rstd_act(dst_ap, src_ap, res_list, scale):
        k.op("act", lambda e: e.activation(out=dst_ap, in_=src_ap, func=AF.Ln, scale=float(scale), bias=epst[:, 0:1]),
             reads=res_list + [epst], writes=res_list)
        k.op("act", lambda e: e.activation(out=dst_ap, in_=dst_ap, func=AF.Exp, scale=-0.5), reads=res_list, writes=res_list)

    psall = nc.alloc_psum_tensor("psall", [128, 8 * 512], F32).ap()
    pbank = [Tl("pb%d" % i, psall[:, i * 512:(i + 1) * 512]) for i in range(8)]

    def bank_bf(b):
        return b.ap.bitcast(BF16)

    modcol = k.sb("modcol", [128, 6, 8, NB], F32)
    A1 = k.sb("A1", [128, NB, 8], F32)
    B1 = k.sb("B1", [128, NB, 8], F32)
    lam_col = k.sb("lam_col", [128, 1], F32)
    nlam_col = k.sb("nlam_col", [128, 1], F32)

    idxw = k.sb("idxw", [128, NMB], I32)
    idxb = k.sb("idxb", [128, NMB], I32)
    idxs = k.sb("idxs", [128, NMB], I32)
    es_w = ExitStack()
    w_in_sb = Tl("w_in_sb", es_w.enter_context(nc.sbuf_tensor("w_in_sb", [128, 8, 2560], BF16)).ap())
    w_out_sb = Tl("w_out_sb", es_w.enter_context(nc.sbuf_tensor("w_out_sb", [128, 8, D], BF16)).ap())
    w_in_v = din("w_in").ap.rearrange("(k p) f -> p k f", p=128)
    for kk in range(8):
        k.dma("pool", w_in_sb[:, kk, :], w_in_v[:, kk, :], writes=[w_in_sb], sem="w_in", max_dma_last_dim=4096, join=(kk > 0))
    w_out_v = din("w_out").ap.rearrange("(k p) f -> p k f", p=128)
    for kk in range(8):
        k.dma("pool", w_out_sb[:, kk, :], w_out_v[:, kk, :], writes=[w_out_sb], sem="w_out", max_dma_last_dim=4096, join=(kk > 0))

    modrow_d = k.dram("modrow_d", [6, NB, 128, D], F32, track_waw=False)
    with ExitStack() as es0:
        def sb0(name, shape, dtype):
            return Tl(name, es0.enter_context(nc.sbuf_tensor(name, list(shape), dtype)).ap())

        cT = sb0("cT_sb", [128, 8, NB], F32)
        scb = sb0("scb", [128, 8, NB], BF16)
        sc_rep = [sb0("sc_rep%d" % b, [128, 8, 128], BF16) for b in range(NB)]
        wsec = [sb0("wsec%d" % i, [128, 8, 1024], BF16) for i in range(2)]
        bT = sb0("bT", [128, 48], F32)
        brow = [sb0("brow%d" % i, [128, 1024], F32) for i in range(2)]
        n1g = sb0("n1g", [128, 8], F32)
        rowtmp = [sb0("rowtmp%d" % b, [128, D], F32) for b in range(2)]
        lv = sb0("lv", [128, 4 * DK], F32)
        lvp = sb0("lvp", [128, 2 * DK], F32)
        lsum = sb0("lsum", [128, 2], F32)

        k.dma("sp", cT.ap, din("cT").ap, writes=[cT], sem="c0")
        k.dma("sp", bT.ap, din("ada_bT").ap, writes=[bT], sem="c1")
        k.dma("sp", n1g.ap, din("n1g_col").ap, writes=[n1g], sem="c2")
        k.dma("sp", lv.ap, din("lamv").ap.to_broadcast([128, 4 * DK]), writes=[lv], sem="c3")
        k.op("act", lambda e: e.activation(out=scb.ap, in_=cT.ap, func=AF.Silu), reads=[cT], writes=[scb])
        for b in range(NB):
            k.op("dve", lambda e, b=b: e.tensor_copy(out=sc_rep[b].ap, in_=scb[:, :, b:b + 1].to_broadcast([128, 8, 128])),
                 reads=[scb], writes=[sc_rep[b]])
        k.op("dve", lambda e: e.tensor_tensor(out=lvp.ap.rearrange("p (a d) -> p a d", a=2),
                                              in0=lv.ap.rearrange("p (a t d) -> p a t d", a=2, t=2)[:, :, 0, :],
                                              in1=lv.ap.rearrange("p (a t d) -> p a t d", a=2, t=2)[:, :, 1, :],
                                              op=ALU.mult), reads=[lv], writes=[lvp])
        k.op("dve", lambda e: e.tensor_reduce(out=lsum.ap, in_=lvp.ap.rearrange("p (a d) -> p a d", a=2),
                                              axis=AX.X, op=ALU.add), reads=[lvp], writes=[lsum])
        k.op("act", lambda e: e.activation(out=lsum.ap, in_=lsum.ap, func=AF.Exp), reads=[lsum], writes=[lsum])
        k.op("dve", lambda e: e.tensor_tensor(out=lam_col.ap, in0=lsum[:, 0:1], in1=lsum[:, 1:2], op=ALU.subtract),
             reads=[lsum], writes=[lam_col])
        k.op("dve", lambda e: e.tensor_scalar(out=lam_col.ap, in0=lam_col.ap, scalar1=float(LAM_INIT), scalar2=None,
                                              op0=ALU.add), reads=[lam_col], writes=[lam_col])
        k.op("dve", lambda e: e.tensor_scalar(out=nlam_col.ap, in0=lam_col.ap, scalar1=-1.0, scalar2=None,
                                              op0=ALU.mult), reads=[lam_col], writes=[nlam_col])

        ada_v = din("ada_w").ap.rearrange("(k p) f -> p k f", p=128)
        for sec in range(6):
            wb = wsec[sec % 2]
            k.dma("pool", wb.ap, ada_v[:, :, sec * 1024:(sec + 1) * 1024], writes=[wb], sem="wsec%d" % (sec % 2))
            if sec in (0, 1):
                pb = pbank[5 + sec % 2]
                pv = pb.ap[:, 0:16].rearrange("p (j b) -> p j b", b=NB)
                for j in range(8):
                    for kk in range(8):
                        first = (j == 0 and kk == 0)
                        last = (j == 7 and kk == 7)
                        k.op("pe", lambda e, j=j, kk=kk, first=first, last=last: e.matmul(
                            pv[:, j, :], lhsT=wb[:, kk, j * 128:(j + 1) * 128], rhs=scb[:, kk, :],
                            start=first, stop=(kk == 7), skip_group_check=True),
                            reads=[wb, scb], writes=[pb], signal=last)
                k.op("dve", lambda e, sec=sec, pv=pv: e.tensor_tensor(
                    out=modcol[:, sec, :, :], in0=pv,
                    in1=bT[:, sec * 8:(sec + 1) * 8].unsqueeze(2).to_broadcast([128, 8, NB]), op=ALU.add),
                    reads=[pb, bT], writes=[modcol])
            else:
                br = brow[sec % 2]
                k.dma("sp", br.ap, din("ada_b").ap[:, sec * 1024:(sec + 1) * 1024].to_broadcast([128, 1024]), writes=[br], sem="brow")
                for b in range(NB):
                    dst = rowtmp[b]
                    for half in range(2):
                        pb = pbank[5 + (b * 2 + half) % 3]
                        for kk in range(8):
                            k.op("pe", lambda e, kk=kk, b=b, half=half, pb=pb: e.matmul(
                                pb.ap, lhsT=sc_rep[b][:, kk, :], rhs=wb[:, kk, half * 512:(half + 1) * 512],
                                start=(kk == 0), stop=(kk == 7)),
                                reads=[wb, sc_rep[b]], writes=[pb], signal=(kk == 7))
                        k.op("dve", lambda e, dst=dst, half=half, pb=pb, br=br: e.tensor_tensor(
                            out=dst[:, half * 512:(half + 1) * 512], in0=pb.ap, in1=br[:, half * 512:(half + 1) * 512],
                            op=ALU.add), reads=[pb, br], writes=[dst])
                    k.dma("sp", modrow_d.ap[sec, b], dst.ap, reads=[dst], writes=[modrow_d], sem="rowst%d" % b)
        for b in range(NB):
            k.op("dve", lambda e, b=b: e.scalar_tensor_tensor(out=A1[:, b, :], in0=modcol[:, 1, :, b], scalar=1.0,
                                                              in1=n1g.ap, op0=ALU.add, op1=ALU.mult),
                 reads=[modcol, n1g], writes=[A1])
            k.op("dve", lambda e, b=b: e.tensor_copy(out=B1[:, b, :], in_=modcol[:, 0, :, b]),
                 reads=[modcol], writes=[B1])
        k_all_quiesce(k)

    if stop_after == "p0":
        dbg = k.sb("dbg", [128, D], F32)
        k.op("dve", lambda e: e.memset(dbg.ap, 0.0), writes=[dbg])
        k.op("dve", lambda e: e.tensor_copy(out=dbg[:, 0:16], in_=A1.ap.rearrange("p b k -> p (b k)")), reads=[A1], writes=[dbg])
        k.op("dve", lambda e: e.tensor_copy(out=dbg[:, 16:32], in_=B1.ap.rearrange("p b k -> p (b k)")), reads=[B1], writes=[dbg])
        k.op("dve", lambda e: e.tensor_copy(out=dbg[:, 32:33], in_=lam_col.ap), reads=[lam_col], writes=[dbg])
        k.dma("sp", out_t.ap[0:128, :], dbg.ap, reads=[dbg], sem="ost")
        for b in range(NB):
            k.dma("sp", dbg.ap, modrow_d.ap[2, b], reads=[modrow_d], writes=[dbg], sem="dbgl")
            k.dma("sp", out_t.ap[128 * (b + 1):128 * (b + 2), :], dbg.ap, reads=[dbg], sem="ost")
        k.finish()
        return nc

    full = stop_after is None
    xs_d = k.dram("xs_d", [PSLOT, D], BF16, track_waw=False)
    xs_init = Res("xs_init")
    wgu_bf_d = k.dram("wgu_bf_d", [NE * 128, 8 * 2 * DFF], BF16, track_waw=False)
    wdn_bf_d = k.dram("wdn_bf_d", [NE * 128, 8 * D], BF16, track_waw=False)

    def convert_expert(e, first, after=()):
        k.dma("pool", wgu_bf_d.ap[e * 128:(e + 1) * 128, :].rearrange("p (k f) -> p k f", k=8),
              din("w_gu").ap[e].rearrange("(k p) f -> p k f", p=128), reads=list(after), writes=[wgu_bf_d], sem="wconv", join=not first)
        k.dma("pool", wdn_bf_d.ap[e * 128:(e + 1) * 128, :].rearrange("p (k f) -> p k f", k=8),
              din("w_dn").ap[e].rearrange("(k p) f -> p k f", p=128), reads=list(after), writes=[wdn_bf_d], sem="wconv", join=True)

    x1_dst = out_t if stop_after == "p1" else x1_d
    with ExitStack() as es1:
        def sb1(name, shape, dtype):
            return Tl(name, es1.enter_context(nc.sbuf_tensor(name, list(shape), dtype)).ap())

        kT_blk = [sb1("kT%d" % i, [128, H, QB], BF16) for i in range(NBLK)]
        V_blk = [sb1("V%d" % i, [128, 4, H, 130], BF16) for i in range(NBLK)]
        xb = [sb1("xb%d" % i, [128, D], F32) for i in range(2)]
        arenaA = sb1("arenaA", [128, 2048], F32)
        xn = alias(arenaA, [4, D], BF16)
        arenaB = sb1("arenaB", [128, 2048], F32)
        hT = alias(arenaB, [8, QB], BF16)
        attnT = alias(arenaB, [4, QB], BF16)
        lru_nb = alias(arenaB, [LRU_C, QB], BF16, off_bytes=4096)
        arenaC = sb1("arenaC", [128, 512], F32)
        junk = alias(arenaC, [D], BF16)
        n_sq = alias(arenaC, [512], F32)
        t_gg = alias(arenaC, [QB], F32)
        qT = sb1("qT", [128, 2, H, QB], BF16)
        Et = [sb1("E%d" % i, [128, 2, QB], BF16) for i in range(2)]
        attn_n = sb1("attn_n", [128, 4, 512], BF16)
        t_mix = alias(attn_n, [D], F32)
        xr_ext = sb1("xr_ext", [128, LRU_C, QB + 4], F32)
        lru_f = sb1("lru_f", [128, LRU_C, QB], BF16)
        blk_f = alias(lru_f, [2, LRU_C, 128], F32)
        x1b = [alias(lru_f, [D], F32)]
        e_o1 = sb1("e_o1", [128, 3, 128], F32)
        e_sq = sb1("e_sq", [128, 3, 128], F32)
        hstate = sb1("hstate", [128, LRU_C], F32)
        t_xc = sb1("t_xc", [128, QB], F32)
        t_xcb = sb1("t_xcb", [128, QB], BF16)
        t_sqb = alias(t_xcb, [QB], BF16)
        t_r = sb1("t_r", [128, QB], F32)
        t_rl = alias(t_r, [QB], F32)
        t_i = sb1("t_i", [128, QB], F32)
        t_a = sb1("t_a", [128, QB], F32)
        t_a2 = sb1("t_a2", [128, QB], F32)
        t_h = sb1("t_h", [128, QB], F32)
        e_t2 = alias(t_h, [3, 128], F32)
        t_sq = sb1("t_sq", [128, QB], F32)
        n_qn_l2 = [[sb1("n_qn%d_%d" % (p, g), [128, 512], BF16) for g in range(2)] for p in range(2)]
        st8_l2 = [[sb1("st8_%d_%d" % (p, g), [128, 8], F32) for g in range(2)] for p in range(2)]
        e_st = sb1("e_st", [128, 12], F32)
        stat = sb1("stat", [128, 16], F32)
        cexp = sb1("cexp", [128, 4], F32)
        qg = sb1("qg", [128, 1], F32)
        kg = sb1("kg", [128, 1], F32)
        subg_bc = sb1("subg_bc", [128, DV], F32)
        convc = sb1("convc", [128, LRU_C, 5], F32)
        blk_b = sb1("blk_b", [128, 2, LRU_C, 128], BF16)
        lvec = sb1("lvec", [128, LRU_C, 4], F32)
        coef = sb1("coef", [128, LRU_C, 2], F32)
        g1_cur = sb1("g1_cur", [128, D], F32)

        k.dma("sp", qg.ap, din("qg_col").ap, writes=[qg], sem="c0")
        k.dma("sp", kg.ap, din("kg_col").ap, writes=[kg], sem="c1")
        k.dma("sp", subg_bc.ap, din("subg_row").ap.to_broadcast([128, DV]), writes=[subg_bc], sem="c2")
        k.dma("sp", convc.ap, din("conv_col").ap, writes=[convc], sem="c3")
        k.dma("sp", blk_f.ap, din("lru_blk").ap, writes=[blk_f], sem="c0")
        k.dma("sp", lvec.ap, din("lru_vec").ap, writes=[lvec], sem="c1")
        k.op("dve", lambda e: e.tensor_copy(out=blk_b.ap, in_=blk_f.ap), reads=[blk_f], writes=[blk_b])
        k.op("dve", lambda e: e.tensor_scalar(out=qg.ap, in0=qg.ap, scalar1=float(DK ** -0.5), scalar2=None, op0=ALU.mult),
             reads=[qg], writes=[qg])
        k.op("dve", lambda e: e.tensor_scalar(out=subg_bc.ap, in0=subg_bc.ap, scalar1=float(1.0 - LAM_INIT), scalar2=None,
                                              op0=ALU.mult), reads=[subg_bc], writes=[subg_bc])
        k.op("dve", lambda e: e.memset(cexp[:, 0:1], -0.5), writes=[cexp])
        k.op("dve", lambda e: e.memset(cexp[:, 1:2], 0.5), writes=[cexp])
        k.op("act", lambda e: e.activation(out=coef[:, :, 0], in_=lvec[:, :, 2], func=AF.Exp, scale=-1.0),
             reads=[lvec], writes=[coef])
        k.op("dve", lambda e: e.tensor_scalar(out=coef[:, :, 0], in0=coef[:, :, 0], scalar1=1.0, scalar2=None, op0=ALU.add),
             reads=[coef], writes=[coef])
        k.op("act", lambda e: e.activation(out=coef[:, :, 0], in_=coef[:, :, 0], func=AF.Ln), reads=[coef], writes=[coef])
        k.op("dve", lambda e: e.tensor_scalar(out=coef[:, :, 1], in0=coef[:, :, 0], scalar1=-16.0, scalar2=None, op0=ALU.mult),
             reads=[coef], writes=[coef])
        k.op("dve", lambda e: e.tensor_scalar(out=coef[:, :, 0], in0=coef[:, :, 0], scalar1=-8.0, scalar2=None, op0=ALU.mult),
             reads=[coef], writes=[coef])
        for i in range(NBLK):
            k.op("pool", lambda e, i=i: e.memset(V_blk[i][:, :, :, 128:130], 1.0), writes=[V_blk[i]])
        k.op("pool", lambda e: e.memset(qT.ap, 0.0), writes=[qT])

        gstate = {"n": 0, "banks": [7]}

        def gbank():
            bl = gstate["banks"]
            b = pbank[bl[gstate["n"] % len(bl)]]
            gstate["n"] += 1
            return b

        def rstd_pow(dst_ap, src_ap, res_list, scale, n):
            rstd_act(dst_ap, src_ap, res_list, scale)

        def acc_ap(c, s):
            if s < 3:
                return pbank[4 + c].ap[:, s * 129:(s + 1) * 129]
            return pbank[6].ap[:, c * 129:(c + 1) * 129]

        def acc_bank(c, s):
            return pbank[4 + c] if s < 3 else pbank[6]

        def lru_stage_a(b, i, c):
            ps_x = gbank()
            for kk in range(8):
                k.op("pe", lambda e, kk=kk: e.matmul(ps_x.ap, lhsT=w_in_sb[:, kk, 1536 + c * 128:1536 + (c + 1) * 128],
                                                    rhs=hT[:, kk, :], start=(kk == 0), stop=(kk == 7)),
                     reads=[w_in_sb, hT], writes=[ps_x], signal=(kk == 7))
            k.op("act", lambda e: e.activation(out=xr_ext[:, c, 3:3 + QB], in_=ps_x.ap, func=AF.Copy),
                 reads=[ps_x], writes=[xr_ext])
            k.op("dve", lambda e: e.tensor_scalar(out=t_xc.ap, in0=xr_ext[:, c, 0:QB], scalar1=convc[:, c, 0:1],
                                                  scalar2=convc[:, c, 4:5], op0=ALU.mult, op1=ALU.add),
                 reads=[xr_ext, convc], writes=[t_xc])
            for j in range(1, 4):
                k.op("dve", lambda e, j=j: e.scalar_tensor_tensor(out=t_xc.ap, in0=xr_ext[:, c, j:j + QB],
                                                                 scalar=convc[:, c, j:j + 1], in1=t_xc.ap,
                                                                 op0=ALU.mult, op1=ALU.add),
                     reads=[xr_ext, convc, t_xc], writes=[t_xc])
            k.op("dve", lambda e: e.tensor_copy(out=xr_ext[:, c, 0:3], in_=xr_ext[:, c, QB:QB + 3]),
                 reads=[xr_ext], writes=[xr_ext])
            k.op("dve", lambda e: e.tensor_copy(out=t_xcb.ap, in_=t_xc.ap), reads=[t_xc], writes=[t_xcb])

        def lru_stage_b(b, i, c):
            ps_g = gbank()
            for kk in range(8):
                k.op("pe", lambda e, kk=kk: e.matmul(ps_g.ap, lhsT=w_in_sb[:, kk, 2048 + c * 128:2048 + (c + 1) * 128],
                                                    rhs=hT[:, kk, :], start=(kk == 0), stop=(kk == 7)),
                     reads=[w_in_sb, hT], writes=[ps_g], signal=(kk == 7))
            k.op("act", lambda e: e.activation(out=t_gg.ap, in_=ps_g.ap, func=AF.Gelu_apprx_tanh),
                 reads=[ps_g], writes=[t_gg])
            ps_r = gbank()
            k.op("pe", lambda e: e.matmul(ps_r.ap, lhsT=blk_b[:, 0, c, :], rhs=t_xcb.ap, start=True, stop=True),
                 reads=[blk_b, t_xcb], writes=[ps_r])
            k.op("act", lambda e: e.activation(out=t_r.ap, in_=ps_r.ap, func=AF.Sigmoid, bias=lvec[:, c, 0:1]),
                 reads=[ps_r, lvec], writes=[t_r])
            ps_i = gbank()
            k.op("pe", lambda e: e.matmul(ps_i.ap, lhsT=blk_b[:, 1, c, :], rhs=t_xcb.ap, start=True, stop=True),
                 reads=[blk_b, t_xcb], writes=[ps_i])
            k.op("act", lambda e: e.activation(out=t_i.ap, in_=ps_i.ap, func=AF.Sigmoid, bias=lvec[:, c, 1:2]),
                 reads=[ps_i, lvec], writes=[t_i])
            k.op("act", lambda e: e.activation(out=t_a.ap, in_=t_r.ap, func=AF.Exp, scale=coef[:, c, 0:1]),
                 reads=[t_r, coef], writes=[t_a])
            k.op("act", lambda e: e.activation(out=t_a2.ap, in_=t_r.ap, func=AF.Exp, scale=coef[:, c, 1:2]),
                 reads=[t_r, coef], writes=[t_a2])
            k.op("act", lambda e: e.activation(out=t_a2.ap, in_=t_a2.ap, func=AF.Ln, scale=-1.0, bias=epst[:, 1:2]),
                 reads=[t_a2, epst], writes=[t_a2])
            k.op("act", lambda e: e.activation(out=t_a2.ap, in_=t_a2.ap, func=AF.Exp, scale=0.5), reads=[t_a2], writes=[t_a2])
            k.op("dve", lambda e: e.tensor_tensor(out=t_i.ap, in0=t_i.ap, in1=t_xc.ap, op=ALU.mult),
                 reads=[t_i, t_xc], writes=[t_i])
            k.op("dve", lambda e: e.tensor_tensor(out=t_i.ap, in0=t_i.ap, in1=t_a2.ap, op=ALU.mult),
                 reads=[t_i, t_a2], writes=[t_i])
            k.op("dve", lambda e: e.tensor_tensor_scan(out=t_h.ap, data0=t_a.ap, data1=t_i.ap, initial=hstate[:, c:c + 1],
                                                       op0=ALU.mult, op1=ALU.add),
                 reads=[t_a, t_i, hstate], writes=[t_h])
            k.op("dve", lambda e: e.tensor_copy(out=hstate[:, c:c + 1], in_=t_h[:, QB - 1:QB]), reads=[t_h], writes=[hstate])
            k.op("dve", lambda e: e.tensor_tensor(out=lru_f[:, c, :], in0=t_h.ap, in1=t_gg.ap, op=ALU.mult),
                 reads=[t_h, t_gg], writes=[lru_f])
            if c == 0:
                k.op("dve", lambda e: e.tensor_tensor(out=t_sq.ap, in0=lru_f[:, c, :], in1=lru_f[:, c, :], op=ALU.mult),
                     reads=[lru_f], writes=[t_sq])
            else:
                k.op("dve", lambda e: e.tensor_tensor(out=t_rl.ap, in0=lru_f[:, c, :], in1=lru_f[:, c, :], op=ALU.mult),
                     reads=[lru_f], writes=[t_rl])
                k.op("dve", lambda e: e.tensor_tensor(out=t_sq.ap, in0=t_sq.ap, in1=t_rl.ap, op=ALU.add),
                     reads=[t_sq, t_rl], writes=[t_sq])

        def lru_finish():
            k.op("dve", lambda e: e.tensor_copy(out=t_sqb.ap, in_=t_sq.ap), reads=[t_sq], writes=[t_sqb])
            ps_n = gbank()
            k.op("pe", lambda e: e.matmul(ps_n.ap, lhsT=ones_bf.ap, rhs=t_sqb.ap, start=True, stop=True),
                 reads=[ones_bf, t_sqb], writes=[ps_n])
            k.op("act", lambda e: e.activation(out=t_rl.ap, in_=ps_n.ap, func=AF.Ln, scale=1.0 / 512.0, bias=epst[:, 0:1]),
                 reads=[ps_n, epst], writes=[t_rl])
            k.op("act", lambda e: e.activation(out=t_rl.ap, in_=t_rl.ap, func=AF.Exp, scale=-0.5), reads=[t_rl], writes=[t_rl])
            for c in range(LRU_C):
                k.op("dve", lambda e, c=c: e.scalar_tensor_tensor(out=lru_nb[:, c, :], in0=lru_f[:, c, :], scalar=lvec[:, c, 3:4],
                                                                 in1=t_rl.ap, op0=ALU.mult, op1=ALU.mult),
                     reads=[lru_f, lvec, t_rl], writes=[lru_nb])

        def attention_head(b, i, h):
            nkb = 4 * i + 4
            pairs = [(kb, c) for kb in range(nkb) for c in range(2)]
            started = set()

            def qk2(m, h=h):
                kb = m
                r = kb - 4 * i
                qlo = max(0, r) * 128
                j = m % 2
                E = Et[j]
                for c in range(2):
                    psb = pbank[2 * j + c]
                    last = (c == 1)
                    k.op("pe", lambda e, c=c, psb=psb: e.matmul(psb[:, qlo:QB], lhsT=kT_blk[kb // 4][:, h, (kb % 4) * 128:(kb % 4 + 1) * 128],
                                                               rhs=qT[:, c, h, qlo:QB], start=True, stop=(r < 0)),
                         reads=[kT_blk[kb // 4], qT], writes=[psb], signal=(last and r < 0))
                    if r >= 0:
                        k.op("pe", lambda e, psb=psb: e.matmul(psb[:, qlo:qlo + 128], lhsT=ident.ap, rhs=negtri.ap, start=False, stop=True),
                             reads=[ident, negtri], writes=[psb], signal=last)
                sv = psall[:, 2 * j * 512:(2 * j + 2) * 512].rearrange("p (c q) -> p c q", c=2)
                k.op("act", lambda e: e.activation(out=E[:, :, qlo:QB], in_=sv[:, :, qlo:QB], func=AF.Exp),
                     reads=[pbank[2 * j], pbank[2 * j + 1]], writes=[E])

            def av2(m):
                kb = m
                r = kb - 4 * i
                s0 = max(0, r)
                E = Et[m % 2]
                for c in range(2):
                    for s in range(s0, 4):
                        bk = acc_bank(c, s)
                        first = bk.name not in started
                        started.add(bk.name)
                        k.op("pe", lambda e, c=c, s=s, first=first: e.matmul(acc_ap(c, s), lhsT=E[:, c, s * 128:(s + 1) * 128],
                                                                           rhs=V_blk[kb // 4][:, kb % 4, h, 0:129],
                                                                           start=first, stop=(kb == nkb - 1), skip_group_check=True),
                             reads=[E, V_blk[kb // 4]], writes=[bk], signal=(c == 1 and s == 3))

            if h == 0:
                qk2(0)
            for m in range(nkb):
                if m + 1 < nkb:
                    qk2(m + 1)
                av2(m)
            if h + 1 < H:
                qk2(0, h + 1)
            for (sl, ns, banks) in ((slice(0, 3), 3, (pbank[4], pbank[5])), (slice(3, 4), 1, (pbank[6], pbank[6]))):
                if ns == 3:
                    a0 = pbank[4].ap[:, 0:387].rearrange("p (s d) -> p s d", d=129)
                    a1 = pbank[5].ap[:, 0:387].rearrange("p (s d) -> p s d", d=129)
                else:
                    a0 = pbank[6].ap[:, 0:129].rearrange("p (s d) -> p s d", d=129)
                    a1 = pbank[6].ap[:, 129:258].rearrange("p (s d) -> p s d", d=129)
                rl = list(set(banks))
                ri0 = e_st[:, 0:ns].unsqueeze(2)
                ri1 = e_st[:, 3:3 + ns].unsqueeze(2)
                k.op("dve", lambda e: e.reciprocal(out=ri0, in_=a0[:, :, 128:129]), reads=rl, writes=[e_st])
                k.op("dve", lambda e: e.reciprocal(out=ri1, in_=a1[:, :, 128:129]), reads=rl, writes=[e_st])
                k.op("dve", lambda e: e.tensor_scalar(out=e_st[:, 3:3 + ns], in0=e_st[:, 3:3 + ns], scalar1=nlam_col[:, 0:1],
                                                      scalar2=None, op0=ALU.mult), reads=[e_st, nlam_col], writes=[e_st])
                k.op("dve", lambda e: e.tensor_tensor(out=e_o1[:, 0:ns, :], in0=a0[:, :, 0:128], in1=ri0.to_broadcast([128, ns, 128]),
                                                      op=ALU.mult), reads=rl + [e_st], writes=[e_o1])
                k.op("dve", lambda e: e.tensor_tensor(out=e_t2[:, 0:ns, :], in0=a1[:, :, 0:128], in1=ri1.to_broadcast([128, ns, 128]),
                                                      op=ALU.mult), reads=rl + [e_st], writes=[e_t2])
                k.op("dve", lambda e: e.tensor_tensor(out=e_o1[:, 0:ns, :], in0=e_o1[:, 0:ns, :], in1=e_t2[:, 0:ns, :], op=ALU.add),
                     reads=[e_o1, e_t2], writes=[e_o1])
                k.op("dve", lambda e: e.tensor_tensor(out=e_sq[:, 0:ns, :], in0=e_o1[:, 0:ns, :], in1=e_o1[:, 0:ns, :], op=ALU.mult),
                     reads=[e_o1], writes=[e_sq])
                k.op("dve", lambda e: e.tensor_reduce(out=e_st[:, 6:6 + ns], in_=e_sq[:, 0:ns, :], axis=AX.X, op=ALU.add),
                     reads=[e_sq], writes=[e_st])
                rstd_pow(e_st[:, 6:6 + ns], e_st[:, 6:6 + ns], [e_st], 1.0 / DV, ns)
                k.op("dve", lambda e: e.tensor_tensor(out=e_o1[:, 0:ns, :], in0=e_o1[:, 0:ns, :],
                                                      in1=e_st[:, 6:6 + ns].unsqueeze(2).to_broadcast([128, ns, 128]), op=ALU.mult),
                     reads=[e_o1, e_st], writes=[e_o1])
                k.op("dve", lambda e: e.tensor_tensor(out=attn_n[:, sl, h * 128:(h + 1) * 128], in0=e_o1[:, 0:ns, :],
                                                       in1=subg_bc.ap.unsqueeze(1).to_broadcast([128, ns, 128]), op=ALU.mult),
                     reads=[e_o1, subg_bc], writes=[attn_n])

        def phase_a(b, i):
            t0 = b * S + i * QB
            for s in range(4):
                xt = xb[s % 2]
                k.dma("sp", xt.ap, x_in.ap[t0 + s * 128:t0 + (s + 1) * 128, :], writes=[xt], sem="xb%d" % (s % 2))
                k.op("act", lambda e, xt=xt, s=s: e.activation(out=junk.ap, in_=xt.ap, func=AF.Square, accum_out=stat[:, s:s + 1]),
                     reads=[xt], writes=[junk, stat])
                rstd_pow(stat[:, s:s + 1], stat[:, s:s + 1], [stat], 1.0 / D, 1)
                k.op("act", lambda e, xt=xt, s=s: e.activation(out=xn[:, s, :], in_=xt.ap, func=AF.Copy, scale=stat[:, s:s + 1]),
                     reads=[xt, stat], writes=[xn])

        blocks = [(bb, ii) for bb in range(NB) for ii in range(NBLK)]
        phase_a(0, 0)
        for b in range(NB):
            k.op("dve", lambda e: e.memset(hstate.ap, 0.0), writes=[hstate])
            k.op("dve", lambda e: e.memset(xr_ext[:, :, 0:3], 0.0), writes=[xr_ext])
            k.dma("sp", g1_cur.ap, modrow_d.ap[2, b], reads=[modrow_d], writes=[g1_cur], sem="g1ld")
            for i in range(NBLK):
                t0 = b * S + i * QB
                gstate["banks"] = list(range(8))
                for kk in range(8):
                    pb = gbank()
                    pv = bank_bf(pb)[:, 0:512].rearrange("p (s t) -> p s t", t=128)
                    for s in range(4):
                        k.op("pe", lambda e, s=s, kk=kk, pv=pv: e.transpose(pv[:, s, :], xn[:, s, kk * 128:(kk + 1) * 128], ident.ap),
                             reads=[xn, ident], writes=[pb], signal=(s == 3))
                    k.op("dve", lambda e, kk=kk, pb=pb: e.tensor_scalar(out=hT[:, kk, :], in0=bank_bf(pb)[:, 0:512],
                                                                       scalar1=A1[:, b, kk:kk + 1], scalar2=B1[:, b, kk:kk + 1],
                                                                       op0=ALU.mult, op1=ALU.add),
                         reads=[pb, A1, B1], writes=[hT])
                def c_mm(s):
                    pbs = []
                    for g in range(3):
                        pb = gbank()
                        for kk in range(8):
                            k.op("pe", lambda e, kk=kk, s=s, g=g, pb=pb: e.matmul(pb.ap, lhsT=hT[:, kk, s * 128:(s + 1) * 128],
                                                                             rhs=w_in_sb[:, kk, g * 512:(g + 1) * 512],
                                                                             start=(kk == 0), stop=(kk == 7)),
                                 reads=[hT, w_in_sb], writes=[pb], signal=(kk == 7))
                        pbs.append(pb)
                    return pbs

                def c_norm(s, pbs):
                    par = s % 2
                    tmps = [(gbank(), st8_l2[par][g], n_qn_l2[par][g]) for g in range(2)]
                    for g in range(2):
                        nsq = tmps[g][0]
                        k.op("act", lambda e, pb=pbs[g], nsq=nsq: e.activation(out=nsq.ap, in_=pb.ap, func=AF.Square), reads=[pbs[g]], writes=[nsq])
                    k.op("act", lambda e, s=s, pb=pbs[2]: e.activation(out=V_blk[i][:, s, :, 0:128],
                                                                in_=pb.ap.rearrange("p (h d) -> p h d", d=128), func=AF.Copy),
                         reads=[pbs[2]], writes=[V_blk[i]])
                    for g in range(2):
                        nsq, st8, n_qn = tmps[g]
                        k.op("dve", lambda e, nsq=nsq, st8=st8: e.tensor_reduce(out=st8.ap, in_=nsq.ap.rearrange("p (g d) -> p g d", d=DK),
                                                              axis=AX.X, op=ALU.add), reads=[nsq], writes=[st8])
                    for g in range(2):
                        st8 = tmps[g][1]
                        k.op("act", lambda e, st8=st8: e.activation(out=st8.ap, in_=st8.ap, func=AF.Ln, scale=1.0 / DK, bias=epst[:, 0:1]),
                             reads=[st8, epst], writes=[st8])
                    for g in range(2):
                        st8 = tmps[g][1]
                        k.op("act", lambda e, st8=st8: e.activation(out=st8.ap, in_=st8.ap, func=AF.Exp, scale=-0.5), reads=[st8], writes=[st8])
                    for g in range(2):
                        nsq, st8, n_qn = tmps[g]
                        k.op("dve", lambda e, pb=pbs[g], st8=st8, n_qn=n_qn: e.tensor_tensor(out=n_qn.ap.rearrange("p (g d) -> p g d", d=DK),
                                                                  in0=pb.ap.rearrange("p (g d) -> p g d", d=DK),
                                                                  in1=st8.ap.unsqueeze(2).to_broadcast([128, 8, DK]), op=ALU.mult),
                             reads=[pbs[g], st8], writes=[n_qn])
                    return tmps

                def c_tr(s, tmps):
                    for g in range(2):
                        n_qn = tmps[g][2]
                        pt = gbank()
                        ptv = bank_bf(pt)[:, 0:512].rearrange("p (h t) -> p h t", t=128)
                        for hh in range(H):
                            k.op("pe", lambda e, hh=hh, ptv=ptv, n_qn=n_qn: e.transpose(ptv[:, hh, :], n_qn[:, hh * 128:(hh + 1) * 128], ident.ap),
                                 reads=[n_qn, ident], writes=[pt], signal=(hh == H - 1))
                        if g == 0:
                            for c in range(2):
                                k.op("dve", lambda e, ptv=ptv, s=s, c=c: e.tensor_scalar(
                                    out=qT[c * 64:(c + 1) * 64, c, :, s * 128:(s + 1) * 128], in0=ptv[c * 64:(c + 1) * 64],
                                    scalar1=qg[c * 64:(c + 1) * 64, 0:1], scalar2=None, op0=ALU.mult),
                                    reads=[pt, qg], writes=[qT])
                        else:
                            k.op("dve", lambda e, ptv=ptv, s=s: e.tensor_scalar(
                                out=kT_blk[i][:, :, s * 128:(s + 1) * 128], in0=ptv, scalar1=kg[:, 0:1], scalar2=None, op0=ALU.mult),
                                reads=[pt, kg], writes=[kT_blk[i]])

                pend = None
                for s in range(4):
                    pbs = c_mm(s)
                    if pend is not None:
                        c_tr(*pend)
                    pend = (s, c_norm(s, pbs))
                c_tr(*pend)
                if full:
                    sched = [0, 1, 1, 2, 2, 3, 3, 4]
                    for ee in range(sched[i]):
                        e_id = b * 16 + sum(sched[:i]) + ee
                        convert_expert(e_id, e_id == 0, after=[kT_blk[i]])
                gstate["banks"] = [7]
                lru_stage_a(b, i, 0)
                for h in range(H):
                    attention_head(b, i, h)
                    lru_stage_b(b, i, h)
                    if h + 1 < H:
                        lru_stage_a(b, i, h + 1)
                gstate["banks"] = list(range(8))
                for cc in range(4):
                    pb = gbank()
                    pv = bank_bf(pb)[:, 0:512].rearrange("p (s t) -> p s t", t=128)
                    for s in range(4):
                        k.op("pe", lambda e, s=s, cc=cc, pv=pv: e.transpose(pv[:, s, :], attn_n[:, s, cc * 128:(cc + 1) * 128], ident.ap),
                             reads=[attn_n, ident], writes=[pb], signal=(s == 3))
                    k.op("act", lambda e, cc=cc, pb=pb: e.activation(out=attnT[:, cc, :], in_=bank_bf(pb)[:, 0:512], func=AF.Copy),
                         reads=[pb], writes=[attnT])
                lru_finish()
                nxt = b * NBLK + i + 1
                if nxt < len(blocks):
                    phase_a(*blocks[nxt])
                for s in range(4):
                    xt = xb[s % 2]
                    k.dma("sp", xt.ap, x_in.ap[t0 + s * 128:t0 + (s + 1) * 128, :], writes=[xt], sem="xb%d" % (s % 2))
                    for half in range(2):
                        pb = gbank()
                        for cc in range(8):
                            lhs = attnT[:, cc, s * 128:(s + 1) * 128] if cc < 4 else lru_nb[:, cc - 4, s * 128:(s + 1) * 128]
                            k.op("pe", lambda e, cc=cc, lhs=lhs, half=half, pb=pb: e.matmul(
                                pb.ap, lhsT=lhs, rhs=w_out_sb[:, cc, half * 512:(half + 1) * 512], start=(cc == 0), stop=(cc == 7)),
                                reads=[attnT, lru_nb, w_out_sb], writes=[pb], signal=(cc == 7))
                        k.op("dve", lambda e, half=half, pb=pb: e.tensor_tensor(out=t_mix[:, half * 512:(half + 1) * 512], in0=pb.ap,
                                                                             in1=g1_cur[:, half * 512:(half + 1) * 512], op=ALU.mult),
                             reads=[pb, g1_cur], writes=[t_mix])
                    k.op("dve", lambda e, xt=xt: e.tensor_tensor(out=xt.ap, in0=xt.ap, in1=t_mix.ap, op=ALU.add),
                         reads=[xt, t_mix], writes=[xt])
                    k.dma("sp", x1_dst.ap[t0 + s * 128:t0 + (s + 1) * 128, :], xt.ap, reads=[xt], writes=[x1_dst], sem="x1st%d" % (s % 2))
        k_all_quiesce(k)

    if stop_after == "p1":
        k.finish()
        return nc

    es_w.close()

    h2_d = k.dram("h2_d", [T, D], BF16, track_waw=False)
    ys_d = k.dram("ys_d", [PSLOT, D], F32, track_waw=False)
    dest_i = k.sb("dest_i", [128, NTILE, TOPK], I32)
    gate_s = k.sb("gate_s", [128, NTILE, TOPK], F32)

    with ExitStack() as es2:
        def sb2(name, shape, dtype):
            return Tl(name, es2.enter_context(nc.sbuf_tensor(name, list(shape), dtype)).ap())

        A2r = [sb2("A2r%d" % b, [128, D], F32) for b in range(NB)]
        B2r = [sb2("B2r%d" % b, [128, D], F32) for b in range(NB)]
        n2row = sb2("n2row", [128, D], F32)
        x1t = [sb2("x1t%d" % i, [128, D], F32) for i in range(2)]
        junk2 = sb2("junk2", [128, D], BF16)
        h2f_l = [sb2("h2f%d" % i, [128, D], F32) for i in range(2)]
        h2T_l = [sb2("h2T%d" % i, [128, 8, 128], BF16) for i in range(2)]
        st2_l = [sb2("st2_%d" % i, [128, 8], F32) for i in range(2)]
        h2b = [sb2("h2b%d" % i, [128, D], BF16) for i in range(2)]
        rw_f = sb2("rw_f", [128, 8, NE], F32)
        rw_b = sb2("rw_b", [128, 8, NE], BF16)
        rb_bc = sb2("rb_bc", [128, NE], F32)
        U_bf = sb2("U_bf", [128, 128], BF16)
        logit = sb2("logit", [128, NTILE, NE], F32)
        mask = sb2("mask", [128, NTILE, NE], F32)
        mask_b = sb2("mask_b", [128, NTILE, NE], BF16)
        pref_b = sb2("pref_b", [128, NTILE + 1, NE], BF16)
        rank = sb2("rank", [128, NTILE, NE], F32)
        G = sb2("G", [128, NTILE, NE], F32)
        tmpA = sb2("tmpA", [128, NTILE, NE], F32)
        tmpB = sb2("tmpB", [128, NTILE, NE], F32)
        mx8 = sb2("mx8", [128, NTILE, 8], F32)
        cexp2 = sb2("cexp2", [128, 2], F32)
        ssum = sb2("ssum", [128, NTILE], F32)
        cnt = sb2("cnt", [128, NE], F32)
        cnt_i = sb2("cnt_i", [128, NE], I32)
        padf = sb2("padf", [128, NE], F32)
        pend = sb2("pend", [128, NE], F32)
        pstart = sb2("pstart", [128, NE], F32)
        ones32 = sb2("ones32", [128, NE], F32)
        jv = sb2("jv", [128, NMB], F32)
        cmpt = sb2("cmpt", [128, NMB, NE], F32)
        bexp_f = sb2("bexp_f", [128, NMB], F32)
        pidx = sb2("pidx", [128, 1], F32)
        same2 = sb2("same2", [128, NMB], F32)
        dest_f = sb2("dest_f", [128, NTILE, TOPK], F32)

        k.op("dve", lambda e: e.memset(cexp2[:, 0:1], -0.5), writes=[cexp2])
        k.dma("sp", n2row.ap, din("n2g_row").ap.to_broadcast([128, D]), writes=[n2row], sem="c0")
        k.dma("sp", rb_bc.ap, din("router_b").ap.to_broadcast([128, NE]), writes=[rb_bc], sem="c1")
        k.dma("sp", rw_f.ap, din("router_w").ap.rearrange("(k p) e -> p k e", p=128), writes=[rw_f], sem="c2")
        k.op("dve", lambda e: e.tensor_copy(out=rw_b.ap, in_=rw_f.ap), reads=[rw_f], writes=[rw_b])
        for b in range(NB):
            k.dma("sp", B2r[b].ap, modrow_d.ap[3, b], reads=[modrow_d], writes=[B2r[b]], sem="c3", join=(b > 0))
            k.dma("sp", A2r[b].ap, modrow_d.ap[4, b], reads=[modrow_d], writes=[A2r[b]], sem="c3", join=True)
        for b in range(NB):
            k.op("dve", lambda e, b=b: e.scalar_tensor_tensor(out=A2r[b].ap, in0=A2r[b].ap, scalar=1.0, in1=n2row.ap,
                                                              op0=ALU.add, op1=ALU.mult), reads=[A2r[b], n2row], writes=[A2r[b]])
        k.op("dve", lambda e: e.tensor_scalar(out=U_bf.ap, in0=iot.ap, scalar1=0.0, scalar2=None, op0=ALU.is_gt),
             reads=[iot], writes=[U_bf])
        k.op("pool", lambda e: e.memset(pref_b[:, 0, :], 0.0), writes=[pref_b])
        k.op("pool", lambda e: e.memset(ones32.ap, 1.0), writes=[ones32])

        p2b = {"n": 0}

        def gb2():
            b = pbank[p2b["n"] % 8]
            p2b["n"] += 1
            return b

        def p2_stage1(t):
            b = t // (NTILE // NB)
            xt = x1t[t % 2]
            hb = h2b[t % 2]
            h2f, h2T, st2 = h2f_l[t % 2], h2T_l[t % 2], st2_l[t % 2]
            k.dma("sp", xt.ap, x1_d.ap[t * 128:(t + 1) * 128, :], reads=[x1_d], writes=[xt], sem="x1t%d" % (t % 2))
            k.op("act", lambda e, xt=xt, st2=st2: e.activation(out=junk2.ap, in_=xt.ap, func=AF.Square, accum_out=st2[:, 0:1]),
                 reads=[xt], writes=[junk2, st2])
            rstd_act(st2[:, 0:1], st2[:, 0:1], [st2], 1.0 / D)
            k.op("dve", lambda e, xt=xt, b=b, h2f=h2f, st2=st2: e.scalar_tensor_tensor(out=h2f.ap, in0=xt.ap, scalar=st2[:, 0:1], in1=A2r[b].ap,
                                                                   op0=ALU.mult, op1=ALU.mult),
                 reads=[xt, st2, A2r[b]], writes=[h2f])
            k.op("dve", lambda e, hb=hb, b=b, h2f=h2f: e.tensor_tensor(out=hb.ap, in0=h2f.ap, in1=B2r[b].ap, op=ALU.add),
                 reads=[h2f, B2r[b]], writes=[hb])
            k.dma("pool", h2_d.ap[t * 128:(t + 1) * 128, :], hb.ap, reads=[hb], writes=[h2_d], sem="h2st%d" % (t % 2))

        def p2_stage1b(t):
            hb = h2b[t % 2]
            h2T = h2T_l[t % 2]
            pb = gb2()
            pv = bank_bf(pb).rearrange("p (k t) -> p k t", t=128)
            for kk in range(8):
                k.op("pe", lambda e, kk=kk, pv=pv, hb=hb: e.transpose(pv[:, kk, :], hb[:, kk * 128:(kk + 1) * 128], ident.ap),
                     reads=[hb, ident], writes=[pb], signal=(kk == 7))
            k.op("act", lambda e, pb=pb, h2T=h2T: e.activation(out=h2T.ap.rearrange("p k t -> p (k t)"), in_=bank_bf(pb), func=AF.Copy),
                 reads=[pb], writes=[h2T])

        def p2_stage2(t):
            h2T = h2T_l[t % 2]
            pl = gb2()
            for kk in range(8):
                k.op("pe", lambda e, kk=kk, pl=pl, h2T=h2T: e.matmul(pl[:, 0:NE], lhsT=h2T[:, kk, :], rhs=rw_b[:, kk, :],
                                                          start=(kk == 0), stop=(kk == 7)),
                     reads=[h2T, rw_b], writes=[pl], signal=(kk == 7))
            k.op("dve", lambda e, pl=pl, t=t: e.tensor_tensor(out=logit[:, t, :], in0=pl[:, 0:NE], in1=rb_bc.ap, op=ALU.add),
                 reads=[pl, rb_bc], writes=[logit])
            k.op("dve", lambda e, t=t: e.max(out=mx8[:, t, :], in_=logit[:, t, :]), reads=[logit], writes=[mx8])
            k.op("dve", lambda e, t=t: e.tensor_scalar(out=mask[:, t, :], in0=logit[:, t, :], scalar1=mx8[:, t, 3:4], scalar2=None,
                                                       op0=ALU.is_ge), reads=[logit, mx8], writes=[mask])
            k.op("dve", lambda e, t=t: e.tensor_copy(out=mask_b[:, t, :], in_=mask[:, t, :]), reads=[mask], writes=[mask_b])
            k.op("dve", lambda e, t=t: e.tensor_tensor(out=pref_b[:, t + 1, :], in0=pref_b[:, t, :], in1=mask_b[:, t, :], op=ALU.add),
                 reads=[pref_b, mask_b], writes=[pref_b])
            pr = gb2()
            k.op("pe", lambda e, pr=pr, t=t: e.matmul(pr[:, 0:NE], lhsT=U_bf.ap, rhs=mask_b[:, t, :], start=True, stop=False),
                 reads=[U_bf, mask_b], writes=[pr], signal=False)
            k.op("pe", lambda e, pr=pr, t=t: e.matmul(pr[:, 0:NE], lhsT=ones_bf.ap, rhs=pref_b[:, t, :], start=False, stop=True),
                 reads=[ones_bf, pref_b], writes=[pr])
            k.op("act", lambda e, pr=pr, t=t: e.activation(out=rank[:, t, :], in_=pr[:, 0:NE], func=AF.Copy), reads=[pr], writes=[rank])

        p2_stage1(0)
        p2_stage1b(0)
        for t in range(NTILE):
            if t + 1 < NTILE:
                p2_stage1(t + 1)
            p2_stage2(t)
            if t + 1 < NTILE:
                p2_stage1b(t + 1)
        pc = gb2()
        k.op("pe", lambda e: e.matmul(pc[:, 0:NE], lhsT=ones_bf.ap, rhs=pref_b[:, NTILE, :], start=True, stop=True),
             reads=[ones_bf, pref_b], writes=[pc])
        k.op("dve", lambda e: e.tensor_scalar(out=cnt.ap, in0=pc[:, 0:NE], scalar1=float(MB - 1), scalar2=None, op0=ALU.add),
             reads=[pc], writes=[cnt])
        k.op("dve", lambda e: e.tensor_copy(out=cnt_i.ap, in_=cnt.ap), reads=[cnt], writes=[cnt_i])
        k.op("dve", lambda e: e.tensor_scalar(out=cnt_i.ap, in0=cnt_i.ap, scalar1=int(math.log2(MB)), scalar2=None, op0=ALU.arith_shift_right),
             reads=[cnt_i], writes=[cnt_i])
        k.op("dve", lambda e: e.tensor_copy(out=padf.ap, in_=cnt_i.ap), reads=[cnt_i], writes=[padf])
        k.op("dve", lambda e: e.tensor_scalar(out=padf.ap, in0=padf.ap, scalar1=float(MB), scalar2=None, op0=ALU.mult),
             reads=[padf], writes=[padf])
        k.op("dve", lambda e: e.tensor_tensor_scan(out=pend.ap, data0=ones32.ap, data1=padf.ap, initial=0.0, op0=ALU.mult, op1=ALU.add),
             reads=[ones32, padf], writes=[pend])
        k.op("dve", lambda e: e.tensor_tensor(out=pstart.ap, in0=pend.ap, in1=padf.ap, op=ALU.subtract),
             reads=[pend, padf], writes=[pstart])
        k.op("pool", lambda e: e.iota(jv.ap, [[MB, NMB]], base=0, channel_multiplier=0, allow_small_or_imprecise_dtypes=True),
             writes=[jv])
        k.op("dve", lambda e: e.tensor_tensor(out=cmpt.ap, in0=pend.ap.unsqueeze(1).to_broadcast([128, NMB, NE]),
                                              in1=jv.ap.unsqueeze(2).to_broadcast([128, NMB, NE]), op=ALU.is_le),
             reads=[pend, jv], writes=[cmpt])
        k.op("dve", lambda e: e.tensor_reduce(out=bexp_f.ap, in_=cmpt.ap, axis=AX.X, op=ALU.add), reads=[cmpt], writes=[bexp_f])
        k.op("dve", lambda e: e.tensor_scalar(out=bexp_f.ap, in0=bexp_f.ap, scalar1=float(NE - 1), scalar2=None, op0=ALU.min),
             reads=[bexp_f], writes=[bexp_f])
        k.op("dve", lambda e: e.tensor_copy(out=idxb.ap, in_=bexp_f.ap), reads=[bexp_f], writes=[idxb])
        k.op("dve", lambda e: e.memset(same2.ap, 0.0), writes=[same2])
        k.op("dve", lambda e: e.tensor_tensor(out=same2[:, 2:NMB], in0=bexp_f[:, 2:NMB], in1=bexp_f[:, 0:NMB - 2], op=ALU.is_equal),
             reads=[bexp_f], writes=[same2])
        k.op("pool", lambda e: e.iota(pidx.ap, [[0, 1]], base=0, channel_multiplier=1, allow_small_or_imprecise_dtypes=True),
             writes=[pidx])
        k.op("dve", lambda e: e.tensor_scalar(out=bexp_f.ap, in0=bexp_f.ap, scalar1=128.0, scalar2=pidx[:, 0:1],
                                              op0=ALU.mult, op1=ALU.add), reads=[bexp_f, pidx], writes=[bexp_f])
        k.op("dve", lambda e: e.tensor_copy(out=idxw.ap, in_=bexp_f.ap), reads=[bexp_f], writes=[idxw])
        k.op("dve", lambda e: e.scalar_tensor_tensor(out=bexp_f.ap, in0=same2.ap, scalar=1.0e6, in1=bexp_f.ap, op0=ALU.mult, op1=ALU.add),
             reads=[same2, bexp_f], writes=[bexp_f])
        k.op("dve", lambda e: e.tensor_copy(out=idxs.ap, in_=bexp_f.ap), reads=[bexp_f], writes=[idxs])
        k.op("dve", lambda e: e.tensor_tensor(out=tmpA.ap, in0=logit.ap, in1=mx8[:, :, 0:1].to_broadcast([128, NTILE, NE]),
                                              op=ALU.subtract), reads=[logit, mx8], writes=[tmpA])
        k.op("act", lambda e: e.activation(out=tmpA.ap, in_=tmpA.ap, func=AF.Exp), reads=[tmpA], writes=[tmpA])
        k.op("dve", lambda e: e.tensor_tensor(out=tmpA.ap, in0=tmpA.ap, in1=mask.ap, op=ALU.mult), reads=[tmpA, mask], writes=[tmpA])
        k.op("dve", lambda e: e.tensor_reduce(out=ssum.ap, in_=tmpA.ap, axis=AX.X, op=ALU.add), reads=[tmpA], writes=[ssum])
        k.op("dve", lambda e: e.reciprocal(out=ssum.ap, in_=ssum.ap), reads=[ssum], writes=[ssum])
        k.op("dve", lambda e: e.tensor_tensor(out=G.ap, in0=tmpA.ap, in1=ssum.ap.unsqueeze(2).to_broadcast([128, NTILE, NE]),
                                              op=ALU.mult), reads=[tmpA, ssum], writes=[G])
        k.op("dve", lambda e: e.tensor_tensor(out=rank.ap, in0=rank.ap, in1=pstart.ap.unsqueeze(1).to_broadcast([128, NTILE, NE]),
                                              op=ALU.add), reads=[rank, pstart], writes=[rank])
        for kq in range(TOPK):
            k.op("dve", lambda e, kq=kq: e.tensor_tensor(out=tmpB.ap, in0=logit.ap,
                                                         in1=mx8[:, :, kq:kq + 1].to_broadcast([128, NTILE, NE]), op=ALU.is_equal),
                 reads=[logit, mx8], writes=[tmpB])
            k.op("dve", lambda e: e.tensor_tensor(out=tmpA.ap, in0=tmpB.ap, in1=rank.ap, op=ALU.mult),
                 reads=[tmpB, rank], writes=[tmpA])
            k.op("dve", lambda e, kq=kq: e.tensor_reduce(out=dest_f[:, :, kq], in_=tmpA.ap, axis=AX.X, op=ALU.add),
                 reads=[tmpA], writes=[dest_f])
            k.op("dve", lambda e: e.tensor_tensor(out=tmpA.ap, in0=tmpB.ap, in1=G.ap, op=ALU.mult),
                 reads=[tmpB, G], writes=[tmpA])
            k.op("dve", lambda e, kq=kq: e.tensor_reduce(out=gate_s[:, :, kq], in_=tmpA.ap, axis=AX.X, op=ALU.add),
                 reads=[tmpA], writes=[gate_s])
        k.op("dve", lambda e: e.tensor_copy(out=dest_i.ap, in_=dest_f.ap), reads=[dest_f], writes=[dest_i])
        hsc = h2b + [sb2("h2sc%d" % i, [128, D], BF16) for i in range(2)]
        for t in range(NTILE):
            hb = hsc[t % 4]
            k.dma("sp", hb.ap, h2_d.ap[t * 128:(t + 1) * 128, :], reads=[h2_d], writes=[hb], sem="h2ld%d" % (t % 4))
            for kq in range(TOPK):
                k.dma("pool", xs_d.ap, hb.ap, reads=[hb, dest_i], writes=[xs_d], sem="scat%d" % (t % 4), join=(kq > 0),
                      indirect=dict(out_offset=bass.IndirectOffsetOnAxis(ap=dest_i[:, t, kq:kq + 1], axis=0), in_offset=None))
        if stop_after == "p2":
            dbg = sb2("dbg2", [128, D], F32)
            k.op("dve", lambda e: e.memset(dbg.ap, 0.0), writes=[dbg])
            k.op("dve", lambda e: e.tensor_copy(out=dbg[:, 0:256], in_=dest_f.ap.rearrange("p t k -> p (t k)")), reads=[dest_f], writes=[dbg])
            k.op("dve", lambda e: e.tensor_copy(out=dbg[:, 256:512], in_=gate_s.ap.rearrange("p t k -> p (t k)")), reads=[gate_s], writes=[dbg])
            k.op("dve", lambda e: e.tensor_copy(out=dbg[:, 512:544], in_=pstart.ap), reads=[pstart], writes=[dbg])
            k.op("dve", lambda e: e.tensor_copy(out=dbg[:, 544:576], in_=pc[:, 0:NE]), reads=[pc], writes=[dbg])
            k.op("dve", lambda e: e.tensor_copy(out=dbg[:, 576:672], in_=bexp_f.ap), reads=[bexp_f], writes=[dbg])
            k.dma("sp", out_t.ap[0:128, :], dbg.ap, reads=[dbg], sem="ost")
            k.dma("sp", out_t.ap[128:256, :], logit.ap.rearrange("p t e -> p (t e)")[:, 0:1024], reads=[logit], sem="ost")
            k.dma("sp", out_t.ap[384:512, :], A2r[0].ap, reads=[A2r[0]], sem="ost")
            k.dma("sp", out_t.ap[512:640, :], B2r[0].ap, reads=[B2r[0]], sem="ost")
            dbg3 = sb2("dbg3", [128, D], F32)
            k.dma("sp", h2b[0].ap, h2_d.ap[0:128, :], reads=[h2_d], writes=[h2b[0]], sem="dbgl")
            k.op("dve", lambda e: e.tensor_copy(out=dbg3.ap, in_=h2b[0].ap), reads=[h2b[0]], writes=[dbg3])
            k.dma("sp", out_t.ap[256:384, :], dbg3.ap, reads=[dbg3], sem="ost")
        k_all_quiesce(k)

    if stop_after == "p2":
        k.finish()
        return nc

    with ExitStack() as es3:
        def sb3(name, shape, dtype):
            return Tl(name, es3.enter_context(nc.sbuf_tensor(name, list(shape), dtype)).ap())

        wgu = [sb3("wgu%d" % i, [128, 8, 2 * DFF], BF16) for i in range(2)]
        wdn = [sb3("wdn%d" % i, [128, 8, D], BF16) for i in range(2)]
        bgu = [sb3("bgu%d" % i, [128, 16], F32) for i in range(2)]
        bdn = [sb3("bdn%d" % i, [128, D], F32) for i in range(2)]
        xsb = [sb3("xsb%d" % i, [128, MB // 128, D], BF16) for i in range(2)]
        xgT_l = [sb3("xgT%d" % i, [128, 8, MB], BF16) for i in range(2)]
        xgT_r = [[Res("xgT%d_%d" % (i, kk)) for kk in range(8)] for i in range(2)]
        actT = sb3("actT", [128, 8, MB], BF16)
        actT_r = [Res("actT%d" % i) for i in range(8)]
        g_sb = [sb3("g_sb%d" % i, [128, MB], F32) for i in range(2)]
        sg_sb = [sb3("sg_sb%d" % i, [128, MB], F32) for i in range(2)]
        u_sb = [sb3("u_sb%d" % i, [128, MB], F32) for i in range(2)]
        ysb = [sb3("ysb%d" % i, [128, D], F32) for i in range(2)]

        bgu_v = din("b_gu_col").ap.rearrange("e p c -> (e p) c")
        reg_bc = nc.gpsimd.alloc_register("reg_bc")
        nc.gpsimd.reg_mov(reg_bc, NE * 128 - 1)
        bdn_v = din("b_dn").ap.rearrange("e o d -> (e o) d")
        p3b = {"n": 0}

        def gb3():
            b = pbank[p3b["n"] % 8]
            p3b["n"] += 1
            return b

        def p3_loads(j):
            wg, wd, bg, bd, xs = wgu[j % 2], wdn[j % 2], bgu[j % 2], bdn[j % 2], xsb[j % 2]
            iw = bass.IndirectOffsetOnAxis(ap=idxw[:, j:j + 1], axis=0)
            ib = bass.IndirectOffsetOnAxis(ap=idxb[:, j:j + 1], axis=0)
            isk = bass.IndirectOffsetOnAxis(ap=idxs[:, j:j + 1], axis=0)
            k.dma("pool", wg.ap.rearrange("p k f -> p (k f)"), wgu_bf_d.ap, reads=[wgu_bf_d, idxs], writes=[wg], sem="wgu%d" % (j % 2),
                  indirect=dict(out_offset=None, in_offset=isk, bounds_check=reg_bc, oob_is_err=False))
            k.dma("pool", wd.ap.rearrange("p k f -> p (k f)"), wdn_bf_d.ap, reads=[wdn_bf_d, idxs], writes=[wd], sem="wdn%d" % (j % 2),
                  indirect=dict(out_offset=None, in_offset=isk, bounds_check=reg_bc, oob_is_err=False))
            k.dma("pool", bg.ap, bgu_v, reads=[idxw], writes=[bg], sem="bgu%d" % (j % 2), indirect=dict(out_offset=None, in_offset=iw))
            k.dma("pool", bd.ap, bdn_v, reads=[idxb], writes=[bd], sem="bdn%d" % (j % 2), indirect=dict(out_offset=None, in_offset=ib))

        def p3_load_x(j):
            xs = xsb[j % 2]
            k.dma("sp", xs.ap, xs_d.ap[j * MB:(j + 1) * MB, :].rearrange("(s p) d -> p s d", p=128), reads=[xs_d], writes=[xs],
                  sem="xsb%d" % (j % 2))

        def p3_transposes(j):
            xs = xsb[j % 2]
            xgT = xgT_l[j % 2]
            xr_ = xgT_r[j % 2]
            for kk in range(8):
                pb = gb3()
                pv = bank_bf(pb)[:, 0:MB].rearrange("p (s t) -> p s t", t=128)
                for s in range(MB // 128):
                    k.op("pe", lambda e, s=s, kk=kk, pv=pv, xs=xs: e.transpose(pv[:, s, :], xs[:, s, kk * 128:(kk + 1) * 128], ident.ap),
                         reads=[xs, ident], writes=[pb], signal=(s == MB // 128 - 1))
                eng = "act" if kk % 2 == 0 else "dve"
                if eng == "act":
                    k.op("act", lambda e, kk=kk, pb=pb, xgT=xgT: e.activation(out=xgT[:, kk, :], in_=bank_bf(pb)[:, 0:MB], func=AF.Copy),
                         reads=[pb], writes=[xr_[kk]])
                else:
                    k.op("dve", lambda e, kk=kk, pb=pb, xgT=xgT: e.tensor_copy(out=xgT[:, kk, :], in_=bank_bf(pb)[:, 0:MB]),
                         reads=[pb], writes=[xr_[kk]])

        p3_loads(0)
        p3_load_x(0)
        p3_load_x(1)
        p3_transposes(0)
        for j in range(NMB):
            wg, wd, bg, bd = wgu[j % 2], wdn[j % 2], bgu[j % 2], bdn[j % 2]
            xgT = xgT_l[j % 2]
            xr_ = xgT_r[j % 2]
            if j + 1 < NMB:
                p3_loads(j + 1)
            for fc in range(8):
                pg = gb3()
                for kk in range(8):
                    k.op("pe", lambda e, kk=kk, fc=fc, pg=pg, wg=wg, xgT=xgT: e.matmul(pg[:, 0:MB], lhsT=wg[:, kk, fc * 128:(fc + 1) * 128], rhs=xgT[:, kk, :],
                                                                          start=(kk == 0), stop=(kk == 7)),
                         reads=[wg, xr_[kk]], writes=[pg], signal=(kk == 7))
                pu = gb3()
                for kk in range(8):
                    k.op("pe", lambda e, kk=kk, fc=fc, pu=pu, wg=wg, xgT=xgT: e.matmul(pu[:, 0:MB], lhsT=wg[:, kk, DFF + fc * 128:DFF + (fc + 1) * 128],
                                                                          rhs=xgT[:, kk, :], start=(kk == 0), stop=(kk == 7)),
                         reads=[wg, xr_[kk]], writes=[pu], signal=(kk == 7))
                gs, sgs, us = g_sb[fc % 2], sg_sb[fc % 2], u_sb[fc % 2]
                k.op("dve", lambda e, pg=pg, gs=gs, bg=bg, fc=fc: e.tensor_scalar(out=gs.ap, in0=pg[:, 0:MB], scalar1=bg[:, fc:fc + 1], scalar2=7.0,
                                                                               op0=ALU.add, op1=ALU.min), reads=[pg, bg], writes=[gs])
                k.op("act", lambda e, gs=gs, sgs=sgs: e.activation(out=sgs.ap, in_=gs.ap, func=AF.Sigmoid, scale=1.702),
                     reads=[gs], writes=[sgs])
                k.op("dve", lambda e, pu=pu, us=us, bg=bg, fc=fc: e.tensor_scalar(out=us.ap, in0=pu[:, 0:MB], scalar1=bg[:, 8 + fc:9 + fc], scalar2=7.0,
                                                                               op0=ALU.add, op1=ALU.min), reads=[pu, bg], writes=[us])
                k.op("dve", lambda e, us=us: e.tensor_scalar(out=us.ap, in0=us.ap, scalar1=-7.0, scalar2=1.0, op0=ALU.max, op1=ALU.add),
                     reads=[us], writes=[us])
                k.op("dve", lambda e, gs=gs, sgs=sgs: e.tensor_tensor(out=gs.ap, in0=gs.ap, in1=sgs.ap, op=ALU.mult),
                     reads=[gs, sgs], writes=[gs])
                k.op("dve", lambda e, gs=gs, us=us, fc=fc: e.tensor_tensor(out=actT[:, fc, :], in0=gs.ap, in1=us.ap, op=ALU.mult),
                     reads=[gs, us], writes=[actT_r[fc]])
            if j + 2 < NMB:
                p3_load_x(j + 2)
            if j + 1 < NMB:
                p3_transposes(j + 1)
            for s in range(MB // 128):
                yt = ysb[s % 2]
                for half in range(2):
                    pd = gb3()
                    for fc in range(8):
                        k.op("pe", lambda e, fc=fc, s=s, half=half, pd=pd, wd=wd: e.matmul(
                            pd.ap, lhsT=actT[:, fc, s * 128:(s + 1) * 128], rhs=wd[:, fc, half * 512:(half + 1) * 512],
                            start=(fc == 0), stop=(fc == 7)), reads=[actT_r[fc], wd], writes=[pd], signal=(fc == 7))
                    k.op("dve", lambda e, half=half, pd=pd, yt=yt, bd=bd: e.tensor_tensor(
                        out=yt[:, half * 512:(half + 1) * 512], in0=pd.ap, in1=bd[:, half * 512:(half + 1) * 512], op=ALU.add),
                        reads=[pd, bd], writes=[yt])
                k.dma("sp", ys_d.ap[j * MB + s * 128:j * MB + (s + 1) * 128, :], yt.ap, reads=[yt], writes=[ys_d], sem="yst%d" % (s % 2))
        k_all_quiesce(k)

    with ExitStack() as es4:
        def sb4(name, shape, dtype):
            return Tl(name, es4.enter_context(nc.sbuf_tensor(name, list(shape), dtype)).ap())

        g2bc = [sb4("g2bc%d" % b, [128, D], F32) for b in range(NB)]
        x1c = [sb4("x1c%d" % i, [128, D], F32) for i in range(2)]
        yk = [[sb4("yk%d_%d" % (i, q), [128, D], F32) for q in range(TOPK)] for i in range(2)]
        acc = [sb4("acc%d" % i, [128, D], F32) for i in range(2)]
        for b in range(NB):
            k.dma("sp", g2bc[b].ap, modrow_d.ap[5, b], reads=[modrow_d], writes=[g2bc[b]], sem="g2ld")
        def p4_loads(t):
            xt = x1c[t % 2]
            k.dma("sp", xt.ap, x1_d.ap[t * 128:(t + 1) * 128, :], reads=[x1_d], writes=[xt], sem="x1c%d" % (t % 2))
            for q in range(TOPK):
                y = yk[t % 2][q]
                k.dma("pool", y.ap, ys_d.ap, reads=[ys_d, dest_i], writes=[y], sem="yk%d_%d" % (t % 2, q),
                      indirect=dict(out_offset=None, in_offset=bass.IndirectOffsetOnAxis(ap=dest_i[:, t, q:q + 1], axis=0)))

        p4_loads(0)
        for t in range(NTILE):
            b = t // (NTILE // NB)
            xt = x1c[t % 2]
            ac = acc[t % 2]
            if t + 1 < NTILE:
                p4_loads(t + 1)
            k.op("dve", lambda e, t=t, ac=ac: e.tensor_scalar(out=ac.ap, in0=yk[t % 2][0].ap, scalar1=gate_s[:, t, 0:1], scalar2=None,
                                                             op0=ALU.mult), reads=[yk[t % 2][0], gate_s], writes=[ac])
            for q in range(1, TOPK):
                k.op("dve", lambda e, t=t, q=q, ac=ac: e.scalar_tensor_tensor(out=ac.ap, in0=yk[t % 2][q].ap, scalar=gate_s[:, t, q:q + 1],
                                                                             in1=ac.ap, op0=ALU.mult, op1=ALU.add),
                     reads=[yk[t % 2][q], gate_s, ac], writes=[ac])
            k.op("dve", lambda e, ac=ac, b=b: e.tensor_tensor(out=ac.ap, in0=ac.ap, in1=g2bc[b].ap, op=ALU.mult),
                 reads=[ac, g2bc[b]], writes=[ac])
            k.op("dve", lambda e, ac=ac, xt=xt: e.tensor_tensor(out=ac.ap, in0=ac.ap, in1=xt.ap, op=ALU.add),
                 reads=[ac, xt], writes=[ac])
            k.dma("sp", out_t.ap[t * 128:(t + 1) * 128, :], ac.ap, reads=[ac], sem="ost%d" % (t % 2))
        k_all_quiesce(k)

    k.finish()
    return nc


def k_all_quiesce(k):
    targets = []
    for en in ("pe", "act", "dve", "pool"):
        E = k.E[en]
        if E["ptok"] is not None:
            raise RuntimeError("pending unsignalled op at quiesce on " + en)
        if E["seq"] > 0:
            targets.append(Tok(E["sem"], E["seq"], en, "S_" + en))
    for name, (sem, cnt) in k.dsem.items():
        targets.append(Tok(sem, cnt, None, name))
    for en in ("pe", "act", "dve", "pool", "sp"):
        E = k.E[en]
        for t in targets:
            if t.eng == en:
                continue
            val = t.val
            if val > E["known"].get(t.key, 0):
                E["eng"].wait_ge(t.sem, val)
                E["known"][t.key] = val


def make_in_maps(inp):
    f = lambda a: np.ascontiguousarray(a, dtype=np.float32)
    x = inp["x"]
    maps = []
    col8 = lambda v: f(v.reshape(8, 128).T)
    lru_blk = np.zeros((128, 2, LRU_C, 128), np.float32)
    for gi, nm in enumerate(("lru_wa", "lru_wx")):
        w = inp[nm][0]
        for c in range(LRU_C):
            for hb in range(2):
                lru_blk[hb * 64:(hb + 1) * 64, gi, c, hb * 64:(hb + 1) * 64] = w[2 * c + hb]
    conv_col = np.zeros((128, LRU_C, 5), np.float32)
    for j in range(4):
        conv_col[:, :, j] = inp["conv_w"][0, j].reshape(LRU_C, 128).T
    conv_col[:, :, 4] = inp["conv_b"][0].reshape(LRU_C, 128).T
    lru_vec = np.zeros((128, LRU_C, 4), np.float32)
    for j, nm in enumerate(("lru_ba", "lru_bx", "lru_lambda", "lru_out_g")):
        lru_vec[:, :, j] = inp[nm][0].reshape(LRU_C, 128).T
    shared = {
        "ada_w": f(inp["ada_w"][0]),
        "ada_bT": f(inp["ada_b"][0].reshape(48, 128).T),
        "ada_b": f(inp["ada_b"][0].reshape(1, -1)),
        "n1g_col": col8(inp["norm1_g"][0]),
        "n2g_row": f(inp["norm2_g"][0].reshape(1, -1)),
        "w_in": f(inp["w_in"][0]),
        "qg_col": f(np.tile(inp["q_norm_g"][0], 2).reshape(128, 1)),
        "kg_col": f(np.tile(inp["k_norm_g"][0], 2).reshape(128, 1)),
        "lamv": f(np.concatenate([inp["lambda_q1"][0], inp["lambda_k1"][0], inp["lambda_q2"][0], inp["lambda_k2"][0]]).reshape(1, -1)),
        "subg_row": f(inp["attn_subln_g"][0].reshape(1, -1)),
        "conv_col": conv_col,
        "lru_blk": lru_blk,
        "lru_vec": lru_vec,
        "w_out": f(inp["w_out"][0]),
        "router_w": f(inp["router_w"][0]),
        "router_b": f(inp["router_b"][0].reshape(1, -1)),
        "w_gu": f(inp["w_gate_up"][0]),
        "b_gu_col": f(inp["b_gate_up"][0].reshape(NE, 16, 128).transpose(0, 2, 1)),
        "w_dn": f(inp["w_down"][0]),
        "b_dn": f(inp["b_down"][0].reshape(NE, 1, D)),
    }
    for c in range(NCORES):
        m = dict(shared)
        m["x"] = f(x[NB * c:NB * (c + 1)].reshape(T, D))
        cc = inp["c"][NB * c:NB * (c + 1)]
        m["cT"] = f(cc.reshape(NB, 8, 128).transpose(2, 1, 0))
        maps.append(m)
    return maps


_PROG_CACHE = {}


def kernel(**inputs):
    stop_after = inputs.pop("_stop_after", None)
    if stop_after not in _PROG_CACHE:
        _PROG_CACHE[stop_after] = build_program(stop_after)
    nc = _PROG_CACHE[stop_after]
    in_maps = [{kk: v for kk, v in m.items() if kk in nc._used_inputs} for m in make_in_maps(inputs)]
    res = run_bass_kernel_spmd(nc, in_maps, core_ids=list(range(NCORES)))
    outs = [np.asarray(r["out"]).reshape(NB, S, D) for r in res.results]
    return np.concatenate(outs, axis=0).astype(np.float32)
```
